# Optimizing a Trainium2 kernel written in Bass

```python
import math
import jax
import jax.numpy as jnp
from jax import lax
import numpy as np

D_MODEL = 2048
BATCH = 16
SEQ = 2048
DEPTH = 1

MLA_HEADS = 8
MLA_Q_RANK = 512
MLA_KV_RANK = 256
MLA_NOPE_DIM = 128
MLA_ROPE_DIM = 64
MLA_V_DIM = 128
ROPE_THETA = 10000.0
SWA_Q_HEADS = 16
SWA_KV_HEADS = 4
SWA_HEAD_DIM = 64
WINDOW = 128
BLOCK = 128
REL_BUCKETS = 32
REL_MAX_DIST = 128
N_BRANCHES = 2
N_EXPERTS = 64
TOP_K = 8
N_GROUPS = 8
TOPK_GROUPS = 4
EXPERT_FF = 512
SHARED_FF = 512
ROUTED_SCALE = 2.5
EXPERT_BLOCK = 256
ALPHA = (2 * DEPTH) ** 0.25
BETA = (8 * DEPTH) ** -0.25
LN_EPS = 1e-5
RMS_EPS = 1e-6

COL_SIZES = (MLA_Q_RANK, MLA_KV_RANK, MLA_ROPE_DIM, SWA_Q_HEADS * SWA_HEAD_DIM,
             SWA_KV_HEADS * SWA_HEAD_DIM, SWA_KV_HEADS * SWA_HEAD_DIM, N_BRANCHES * D_MODEL)
IN_COLS = sum(COL_SIZES)

kernel_name = "hybrid_mla_swa_gated_moe_deepnorm"


def layer_norm(x, g, b):
    xf = x.astype(jnp.float32)
    mu = jnp.mean(xf, -1, keepdims=True)
    var = jnp.mean(jnp.square(xf - mu), -1, keepdims=True)
    return ((xf - mu) * lax.rsqrt(var + LN_EPS) * g.astype(jnp.float32) + b.astype(jnp.float32)).astype(x.dtype)


def rms_norm(x, g):
    xf = x.astype(jnp.float32)
    return (xf * lax.rsqrt(jnp.mean(jnp.square(xf), -1, keepdims=True) + RMS_EPS) * g.astype(jnp.float32)).astype(x.dtype)


def rope(x, positions):
    d = x.shape[-1]
    inv = ROPE_THETA ** (-jnp.arange(0, d, 2, dtype=jnp.float32) / d)
    ang = positions.astype(jnp.float32)[..., None] * inv
    cos = jnp.cos(ang)[:, :, None, :]
    sin = jnp.sin(ang)[:, :, None, :]
    x1, x2 = jnp.split(x.astype(jnp.float32), 2, axis=-1)
    return jnp.concatenate([x1 * cos - x2 * sin, x1 * sin + x2 * cos], -1).astype(x.dtype)


def t5_bucket(dist):
    n = jnp.maximum(dist, 0)
    max_exact = REL_BUCKETS // 2
    large = max_exact + (jnp.log(jnp.maximum(n, 1).astype(jnp.float32) / max_exact)
                         / math.log(REL_MAX_DIST / max_exact) * (REL_BUCKETS - max_exact)).astype(jnp.int32)
    large = jnp.minimum(large, REL_BUCKETS - 1)
    return jnp.where(n < max_exact, n, large)


def causal_block_attention(q, k, v, scale):
    B, S, H, dk = q.shape
    nb = S // BLOCK
    qb = q.reshape(B, nb, BLOCK, H, dk).transpose(1, 0, 2, 3, 4)
    kpos = jnp.arange(S)

    def one_block(args):
        qi, i = args
        s = jnp.einsum('bqhd,bkhd->bhqk', qi, k, preferred_element_type=jnp.float32) * scale
        qpos = i * BLOCK + jnp.arange(BLOCK)
        s = jnp.where(kpos[None, :] <= qpos[:, None], s, -jnp.inf)
        p = jax.nn.softmax(s, axis=-1).astype(v.dtype)
        return jnp.einsum('bhqk,bkhd->bqhd', p, v)

    out = lax.map(one_block, (qb, jnp.arange(nb)))
    return out.transpose(1, 0, 2, 3, 4).reshape(B, S, H, v.shape[-1])


def swa_sink_attention(q, k, v, sinks, rel_table):
    B, S, HQ, dh = q.shape
    G = HQ // SWA_KV_HEADS
    nb = S // BLOCK
    qb = q.reshape(B, nb, BLOCK, SWA_KV_HEADS, G, dh)

    def band(t):
        tb = t.reshape(B, nb, BLOCK, SWA_KV_HEADS, dh)
        prev = jnp.pad(tb[:, :-1], ((0, 0), (1, 0), (0, 0), (0, 0), (0, 0)))
        return jnp.concatenate([prev, tb], axis=2)

    kb, vb = band(k), band(v)
    s = jnp.einsum('bnqhgd,bnkhd->bnhgqk', qb, kb, preferred_element_type=jnp.float32) * (dh ** -0.5)
    qi = jnp.arange(BLOCK)[:, None]
    kj = jnp.arange(2 * BLOCK)[None, :]
    dist = qi + BLOCK - kj
    bias = rel_table[t5_bucket(dist)].astype(jnp.float32)
    bias = bias.transpose(2, 0, 1).reshape(SWA_KV_HEADS, G, BLOCK, 2 * BLOCK)
    in_window = (dist >= 0) & (dist < WINDOW)
    has_prev = (jnp.arange(nb)[:, None, None] > 0) | (kj[None] >= BLOCK)
    mask = in_window[None] & has_prev
    s = jnp.where(mask[None, :, None, None], s + bias, -jnp.inf)
    sink = jnp.broadcast_to(sinks.astype(jnp.float32).reshape(SWA_KV_HEADS, G, 1, 1), s.shape[:-1] + (1,))
    p = jax.nn.softmax(jnp.concatenate([s, sink], axis=-1), axis=-1)[..., :-1].astype(v.dtype)
    o = jnp.einsum('bnhgqk,bnkhd->bnqhgd', p, vb)
    return o.reshape(B, S, HQ, dh)


def token_mixer(x, positions, w_in, b_gate, q_norm_g, kv_norm_g, w_uq, w_uk, w_uv,
                swa_sinks, rel_table, w_br_mla, w_br_swa, w_out):
    B, S, _ = x.shape
    proj = x @ w_in
    bounds = np.cumsum(COL_SIZES)[:-1].tolist()
    c_q, c_kv, k_r, q_s, k_s, v_s, gate = jnp.split(proj, bounds, axis=-1)

    q = (rms_norm(c_q, q_norm_g) @ w_uq).reshape(B, S, MLA_HEADS, MLA_NOPE_DIM + MLA_ROPE_DIM)
    q_nope, q_rope = jnp.split(q, [MLA_NOPE_DIM], axis=-1)
    c_kv = rms_norm(c_kv, kv_norm_g)
    k_nope = (c_kv @ w_uk).reshape(B, S, MLA_HEADS, MLA_NOPE_DIM)
    v_m = (c_kv @ w_uv).reshape(B, S, MLA_HEADS, MLA_V_DIM)
    k_rope = rope(k_r[:, :, None, :], positions)
    q_m = jnp.concatenate([q_nope, rope(q_rope, positions)], axis=-1)
    k_m = jnp.concatenate([k_nope, jnp.broadcast_to(k_rope, (B, S, MLA_HEADS, MLA_ROPE_DIM))], axis=-1)
    o_m = causal_block_attention(q_m, k_m, v_m, (MLA_NOPE_DIM + MLA_ROPE_DIM) ** -0.5)

    o_s = swa_sink_attention(q_s.reshape(B, S, SWA_Q_HEADS, SWA_HEAD_DIM),
                             k_s.reshape(B, S, SWA_KV_HEADS, SWA_HEAD_DIM),
                             v_s.reshape(B, S, SWA_KV_HEADS, SWA_HEAD_DIM),
                             swa_sinks, rel_table)

    y_m = o_m.reshape(B, S, -1) @ w_br_mla
    y_s = o_s.reshape(B, S, -1) @ w_br_swa
    g = jax.nn.sigmoid(gate.reshape(B, S, N_BRANCHES, D_MODEL) + b_gate)
    merged = g[:, :, 0] * y_m + g[:, :, 1] * y_s
    return merged @ w_out


def moe_ffn(x, w_router, router_bias, w_gate_up, w_down, w_shared_gate_up, w_shared_down):
    B, S, D = x.shape
    xt = x.reshape(-1, D)
    T = xt.shape[0]
    scores = jax.nn.sigmoid((xt @ w_router).astype(jnp.float32))
    sel = scores + router_bias.astype(jnp.float32)
    grp_score = lax.top_k(sel.reshape(T, N_GROUPS, -1), 2)[0].sum(-1)
    _, top_g = lax.top_k(grp_score, TOPK_GROUPS)
    gmask = jax.nn.one_hot(top_g, N_GROUPS).sum(-2) > 0
    emask = jnp.repeat(gmask, N_EXPERTS // N_GROUPS, axis=-1)
    _, top_e = lax.top_k(jnp.where(emask, sel, -jnp.inf), TOP_K)
    w = jnp.take_along_axis(scores, top_e, axis=-1)
    w = w / jnp.sum(w, -1, keepdims=True) * ROUTED_SCALE

    N = T * TOP_K
    NB = -(-N // EXPERT_BLOCK) + N_EXPERTS
    NP = NB * EXPERT_BLOCK
    flat_e = top_e.reshape(-1)
    order = jnp.argsort(flat_e)
    sorted_e = flat_e[order]
    gs = jnp.bincount(flat_e, length=N_EXPERTS)
    ps = (gs + EXPERT_BLOCK - 1) // EXPERT_BLOCK * EXPERT_BLOCK
    start = jnp.cumsum(gs) - gs
    pend = jnp.cumsum(ps)
    pstart = pend - ps
    dest = pstart[sorted_e] + jnp.arange(N) - start[sorted_e]
    row_tok = jnp.full((NP,), T, jnp.int32).at[dest].set((order // TOP_K).astype(jnp.int32))
    row_w = jnp.zeros((NP,), jnp.float32).at[dest].set(w.reshape(-1)[order])
    block_e = jnp.minimum(jnp.searchsorted(pend, jnp.arange(NB) * EXPERT_BLOCK, side='right'), N_EXPERTS - 1)
    x_pad = jnp.concatenate([xt, jnp.zeros((1, D), xt.dtype)], axis=0)

    def expert_block(args):
        toks, wts, e = args
        h = x_pad[toks] @ w_gate_up[e]
        hg, hu = jnp.split(h, 2, axis=-1)
        return ((jax.nn.silu(hg) * hu) @ w_down[e]) * wts[:, None].astype(xt.dtype)

    ys = lax.map(expert_block, (row_tok.reshape(NB, EXPERT_BLOCK), row_w.reshape(NB, EXPERT_BLOCK), block_e))
    routed = jax.ops.segment_sum(ys.reshape(NP, D), row_tok, num_segments=T + 1)[:T]

    sg, su = jnp.split(xt @ w_shared_gate_up, 2, axis=-1)
    shared = (jax.nn.silu(sg) * su) @ w_shared_down
    return (routed + shared).reshape(B, S, D)


def setup_inputs(seed: int = 0) -> dict:
    key = jax.random.key(seed)
    ks = iter(jax.random.split(key, 32))
    f32 = jnp.float32
    L, D = DEPTH, D_MODEL

    def nrm(shape, scale):
        return jax.random.normal(next(ks), shape, f32) * scale

    x = nrm((BATCH, SEQ, D), 1.0)
    positions = jnp.broadcast_to(jnp.arange(SEQ, dtype=jnp.int32), (BATCH, SEQ))
    col_scale = jnp.concatenate([jnp.full((c,), BETA if i == 5 else 1.0, f32) for i, c in enumerate(COL_SIZES)])
    w_in = nrm((L, D, IN_COLS), D ** -0.5) * col_scale
    b_gate = nrm((L, N_BRANCHES, D), 0.1)
    q_norm_g = 1.0 + nrm((L, MLA_Q_RANK), 0.01)
    kv_norm_g = 1.0 + nrm((L, MLA_KV_RANK), 0.01)
    w_uq = nrm((L, MLA_Q_RANK, MLA_HEADS * (MLA_NOPE_DIM + MLA_ROPE_DIM)), MLA_Q_RANK ** -0.5)
    w_uk = nrm((L, MLA_KV_RANK, MLA_HEADS * MLA_NOPE_DIM), MLA_KV_RANK ** -0.5)
    w_uv = nrm((L, MLA_KV_RANK, MLA_HEADS * MLA_V_DIM), MLA_KV_RANK ** -0.5 * BETA)
    swa_sinks = nrm((L, SWA_Q_HEADS), 0.5)
    rel_table = nrm((REL_BUCKETS, SWA_Q_HEADS), 0.5)
    w_br_mla = nrm((L, MLA_HEADS * MLA_V_DIM, D), (MLA_HEADS * MLA_V_DIM) ** -0.5)
    w_br_swa = nrm((L, SWA_Q_HEADS * SWA_HEAD_DIM, D), (SWA_Q_HEADS * SWA_HEAD_DIM) ** -0.5)
    w_out = nrm((L, D, D), D ** -0.5 * BETA)
    ln1_g = 1.0 + nrm((L, D), 0.01)
    ln1_b = nrm((L, D), 0.01)
    w_router = nrm((L, D, N_EXPERTS), D ** -0.5)
    router_bias = nrm((L, N_EXPERTS), 0.01)
    w_gate_up = nrm((L, N_EXPERTS, D, 2 * EXPERT_FF), D ** -0.5)
    w_down = nrm((L, N_EXPERTS, EXPERT_FF, D), EXPERT_FF ** -0.5 * BETA)
    w_shared_gate_up = nrm((L, D, 2 * SHARED_FF), D ** -0.5)
    w_shared_down = nrm((L, SHARED_FF, D), SHARED_FF ** -0.5 * BETA)
    ln2_g = 1.0 + nrm((L, D), 0.01)
    ln2_b = nrm((L, D), 0.01)
    return {"x": x, "positions": positions, "w_in": w_in, "b_gate": b_gate,
            "q_norm_g": q_norm_g, "kv_norm_g": kv_norm_g, "w_uq": w_uq, "w_uk": w_uk,
            "w_uv": w_uv, "swa_sinks": swa_sinks, "rel_table": rel_table,
            "w_br_mla": w_br_mla, "w_br_swa": w_br_swa, "w_out": w_out,
            "ln1_g": ln1_g, "ln1_b": ln1_b, "w_router": w_router, "router_bias": router_bias,
            "w_gate_up": w_gate_up, "w_down": w_down, "w_shared_gate_up": w_shared_gate_up,
            "w_shared_down": w_shared_down, "ln2_g": ln2_g, "ln2_b": ln2_b}


def reference(x, positions, w_in, b_gate, q_norm_g, kv_norm_g, w_uq, w_uk, w_uv, swa_sinks,
              rel_table, w_br_mla, w_br_swa, w_out, ln1_g, ln1_b, w_router, router_bias,
              w_gate_up, w_down, w_shared_gate_up, w_shared_down, ln2_g, ln2_b):
    h = x
    for l in range(DEPTH):
        mix = token_mixer(h, positions, w_in[l], b_gate[l], q_norm_g[l], kv_norm_g[l], w_uq[l],
                          w_uk[l], w_uv[l], swa_sinks[l], rel_table, w_br_mla[l], w_br_swa[l], w_out[l])
        h = layer_norm(ALPHA * h + mix, ln1_g[l], ln1_b[l])
        ffn = moe_ffn(h, w_router[l], router_bias[l], w_gate_up[l], w_down[l],
                      w_shared_gate_up[l], w_shared_down[l])
        h = layer_norm(ALPHA * h + ffn, ln2_g[l], ln2_b[l])
    return h
```

```python
import math
import numpy as np
from contextlib import ExitStack
import concourse.bass as bass
import concourse.mybir as mybir
from concourse.bass_utils import run_bass_kernel_spmd

F32 = mybir.dt.float32; BF16 = mybir.dt.bfloat16; I32 = mybir.dt.int32; U32 = mybir.dt.uint32
AF = mybir.ActivationFunctionType; ALU = mybir.AluOpType; AX = mybir.AxisListType

D = 2048; KC = 16
OFF_CQ, OFF_CKV, OFF_KR, OFF_QS, OFF_KS, OFF_VS, OFF_GATE = 0, 512, 768, 832, 1856, 2112, 2368
NA = 2368
IN_COLS = 6464
NE = 64; FF = 512; TOPK = 8
ALPHA = 2.0 ** 0.25
LN_EPS = 1e-5; RMS_EPS = 1e-6
NEG = -30000.0
TWO_PI = 2.0 * math.pi


def A(*a, **kw):
    return (a, kw)


def _bind(fn, args):
    if isinstance(fn, str):
        a, kw = args
        return lambda e: getattr(e, fn)(*a, **kw)
    return fn


class Buf:
    __slots__ = ("name", "writers", "readers", "gen_deps")
    def __init__(self, name=""):
        self.name = name; self.writers = []; self.readers = []; self.gen_deps = set()


class Prog:
    ENGS = ("tensor", "vector", "scalar", "gpsimd", "sync")
    NDMA = 14
    def __init__(self, nc, stack):
        self.nc = nc
        self.ops = {e: [] for e in self.ENGS}
        self.sem = {e: stack.enter_context(nc.semaphore("c_" + e)) for e in self.ENGS}
        self.cnt = {e: 0 for e in self.ENGS}
        self.dsem = {}; self.duse = {}; self.dnext = {}
        for q in ("sync", "gpsimd", "scalar"):
            self.dsem[q] = [stack.enter_context(nc.semaphore(f"d_{q}{i}")) for i in range(self.NDMA)]
            self.duse[q] = [0] * self.NDMA
            self.dnext[q] = 0
        self.waited = {e: {} for e in self.ENGS}
        self.pending = {e: set() for e in self.ENGS}
        self.nops = 0
    def _deps(self, eng, reads, writes, extra, joins=()):
        deps = set(t for t in extra if t is not None)
        for b in reads:
            deps.update(b.writers)
        for b in writes:
            deps.update(b.writers); deps.update(b.readers)
        for b in joins:
            deps.update(b.gen_deps); deps.update(b.readers)
        if self.pending[eng]:
            deps |= self.pending[eng]; self.pending[eng] = set()
        deps = {t for t in deps if not (t[0] == "c" and t[1] == eng and t[2] > self.cnt[eng])}
        return deps
    def _commit(self, tok, reads, writes, joins=()):
        for b in reads: b.readers.append(tok)
        for b in writes:
            b.gen_deps = set(b.writers) | set(b.readers)
            b.writers = [tok]; b.readers = []
        for b in joins:
            b.writers.append(tok)
    def op(self, eng, fn, args=None, reads=(), writes=(), signal=True, deps=(), joins=()):
        fn = _bind(fn, args)
        d = self._deps(eng, reads, writes, deps, joins)
        if signal:
            self.cnt[eng] += 1
            tok = ("c", eng, self.cnt[eng])
        else:
            tok = ("c", eng, self.cnt[eng] + 1)
        self.ops[eng].append(("op", fn, d, signal))
        self._commit(tok, reads, writes, joins)
        self.nops += 1
        return tok
    def dma(self, q, fn, args=None, reads=(), writes=(), deps=(), joins=()):
        fn = _bind(fn, args)
        d = self._deps(q, reads, writes, deps, joins)
        i = self.dnext[q]; self.dnext[q] = (i + 1) % self.NDMA
        prev = self.duse[q][i]
        if prev > 0: d.add(("d", q, i, prev * 16))
        self.duse[q][i] = prev + 1
        tok = ("d", q, i, (prev + 1) * 16)
        self.ops[q].append(("dma", fn, d, (q, i)))
        self._commit(tok, reads, writes, joins)
        self.nops += 1
        return tok
    def all_tokens(self, final=True):
        toks = set()
        for e in self.ENGS:
            if self.cnt[e] > 0: toks.add(("c", e, self.cnt[e]))
        for q in self.dsem:
            if q == "scalar" and not final: continue
            for i in range(self.NDMA):
                if self.duse[q][i] > 0: toks.add(("d", q, i, self.duse[q][i] * 16))
        return toks
    def flush(self, final=False):
        toks = self.all_tokens(final)
        with self.nc.Block() as block:
            def build(engname):
                def body(eng):
                    waited = self.waited[engname]
                    def do_wait(tok):
                        if tok[0] == "c":
                            key = ("c", tok[1]); val = tok[2]; sem = self.sem[tok[1]]
                        else:
                            key = ("d", tok[1], tok[2]); val = tok[3]; sem = self.dsem[tok[1]][tok[2]]
                        if waited.get(key, 0) >= val: return
                        waited[key] = val
                        eng.wait_ge(sem, val)
                    for kind, fn, deps, info in self.ops[engname]:
                        for tok in sorted(deps, key=str): do_wait(tok)
                        ins = fn(eng)
                        if kind == "op":
                            if info: ins.then_inc(self.sem[engname], 1)
                        else:
                            q, i = info
                            ins.then_inc(self.dsem[q][i], 16)
                    if final and engname == "sync":
                        for tok in sorted(toks, key=str): do_wait(tok)
                return body
            block.tensor(build("tensor")); block.vector(build("vector")); block.scalar(build("scalar"))
            block.gpsimd(build("gpsimd")); block.sync(build("sync"))
        self.ops = {e: [] for e in self.ENGS}
        for e in self.ENGS: self.pending[e] = set(toks)


class PsumBanks:
    def __init__(self, nc, stack, n=8):
        self.t = [stack.enter_context(nc.psum_tensor(f"bank{i}", [128, 512], F32)) for i in range(n)]
        self.b = [Buf(f"bank{i}") for i in range(n)]
        self.i = 0; self.n = n; self.held = set()
    def get(self, hold=False):
        while self.i in self.held:
            self.i = (self.i + 1) % self.n
        i = self.i; self.i = (i + 1) % self.n
        if hold: self.held.add(i)
        return self.t[i], self.b[i]
    def release(self, buf):
        self.held.discard(self.b.index(buf))


def t5_bucket_np(n):
    n = np.maximum(n, 0)
    max_exact = 16
    large = max_exact + (np.log(np.maximum(n, 1).astype(np.float32) / np.float32(max_exact))
                         / np.float32(math.log(128 / max_exact)) * np.float32(32 - max_exact)).astype(np.int32)
    large = np.minimum(large, 31)
    return np.where(n < max_exact, n, large)


def make_consts():
    c = np.zeros((128, 512), np.float32)
    c[:, 0:128] = np.eye(128, dtype=np.float32)
    k = np.arange(128)[:, None]; q = np.arange(128)[None, :]
    c[:, 128:256] = (k <= q).astype(np.float32)
    inv = (10000.0 ** (-np.arange(0, 64, 2, dtype=np.float32) / 64)).astype(np.float32)
    c[:, 256] = np.tile(inv, 4)
    oh = np.zeros((32, 128), np.float32)
    oh[t5_bucket_np(np.arange(128)), np.arange(128)] = 1.0
    c[0:32, 257:385] = oh
    c[:, 385] = np.arange(128, dtype=np.float32)
    return c


def phase_W(k, part, st_ext=None):
    nc, P = k.nc, k.P
    with ExitStack() as st_own:
        st = st_own if st_ext is None else st_ext
        dq = "sync" if part == 1 else "gpsimd"
        CB = 3232
        stg = [st.enter_context(nc.sbuf_tensor(f"w{part}_stg{i}", [128, CB], F32)) for i in range(3)]
        outb = [st.enter_context(nc.sbuf_tensor(f"w{part}_out{i}", [128, CB], BF16)) for i in range(3)]
        rot = [st.enter_context(nc.sbuf_tensor(f"w{part}_rot{i}", [128, 512], BF16)) for i in range(2)]
        b_stg = [Buf() for _ in range(3)]; b_out = [Buf() for _ in range(3)]; b_rot = [Buf() for _ in range(2)]
        engs = ["vector", "gpsimd", "scalar"] if part == 1 else ["gpsimd", "gpsimd", "gpsimd"]
        step = [0]; rstep = [0]; pend = []
        def cast(eng, out, in_):
            if eng == "scalar":
                return "activation", A(out=out, in_=in_, func=AF.Copy)
            return "tensor_copy", A(out=out, in_=in_)
        def do(src, dst, R, C, special=None, dstfn=None):
            nrc = (R + 127) // 128
            for rc in range(nrc):
                r0 = rc * 128; pr = min(128, R - r0)
                for c0 in range(0, C, CB):
                    cw = min(CB, C - c0)
                    i = step[0] % 3; step[0] += 1
                    P.dma(dq, "dma_start", A(out=stg[i][0:pr, 0:cw], in_=src[r0:r0 + pr, c0:c0 + cw]),
                          writes=[b_stg[i]])
                    def finish(i=i, r0=r0, pr=pr, c0=c0, cw=cw, rc=rc):
                        P.op(engs[i], *cast(engs[i], outb[i][0:pr, 0:cw], stg[i][0:pr, 0:cw]), reads=[b_stg[i]], writes=[b_out[i]])
                        if dstfn is None:
                            P.dma(dq, "dma_start", A(out=dst[r0:r0 + pr, c0:c0 + cw], in_=outb[i][0:pr, 0:cw]), reads=[b_out[i]])
                        else:
                            P.dma(dq, "dma_start", A(out=dstfn(rc, c0, cw), in_=outb[i][0:pr, 0:cw].rearrange("p (c n) -> p c n", n=128)), reads=[b_out[i]])
                        if special is not None and c0 == 0:
                            special(i, r0)
                    if pend: pend.pop()()
                    pend.append(finish)
        def sp_in(i, r0):
            j = rstep[0] % 2; rstep[0] += 1
            P.op("vector", "tensor_scalar", A(out=rot[j][:, 0:32], in0=stg[i][:, OFF_KR + 32:OFF_KR + 64], scalar1=-1.0, scalar2=None, op0=ALU.mult),
                 reads=[b_stg[i]], writes=[b_rot[j]])
            P.op("vector", "tensor_copy", A(out=rot[j][:, 32:64], in_=stg[i][:, OFF_KR:OFF_KR + 32]), reads=[b_stg[i]], writes=[b_rot[j]])
            P.dma(dq, "dma_start", A(out=k.wb_krrot[r0:r0 + 128, :], in_=rot[j][:, 0:64]), reads=[b_rot[j]])
        def sp_uq(i, r0):
            j = rstep[0] % 2; rstep[0] += 1
            sv = stg[i][:, 0:1536].rearrange("p (h d) -> p h d", d=192)
            rv = rot[j][:, 0:512].rearrange("p (h d) -> p h d", d=64)
            P.op("vector", "tensor_scalar", A(out=rv[:, :, 0:32], in0=sv[:, :, 160:192], scalar1=-1.0, scalar2=None, op0=ALU.mult),
                 reads=[b_stg[i]], writes=[b_rot[j]])
            P.op("vector", "tensor_copy", A(out=rv[:, :, 32:64], in_=sv[:, :, 128:160]), reads=[b_stg[i]], writes=[b_rot[j]])
            P.dma(dq, "dma_start", A(out=k.wb_uqrot[r0:r0 + 128, :], in_=rot[j][:, 0:512]), reads=[b_rot[j]])
        if part == 1:
            do(k.w_in[:, 0:NA], k.wb_in[:, 0:NA], D, NA, sp_in)
            do(k.w_uq, k.wb_uq, 512, 1536, sp_uq)
            do(k.w_uk, k.wb_uk, 256, 1024); do(k.w_uv, k.wb_uv, 256, 1024)
            if pend: pend.pop()()
            P.flush()
            return
        for half in range(2):
            do(k.w_in[:, OFF_GATE + half * D:OFF_GATE + (half + 1) * D], None, D, D,
               dstfn=lambda rc, c0, cw, half=half: k.wg_d[half * KC + c0 // 128: half * KC + (c0 + cw) // 128, :, rc, :].rearrange("c p n -> p c n"))
        do(k.w_br_mla, None, 1024, D, dstfn=lambda rc, c0, cw: k.wbm_d[c0 // 128:(c0 + cw) // 128, :, rc, :].rearrange("c p n -> p c n"))
        do(k.w_br_swa, None, 1024, D, dstfn=lambda rc, c0, cw: k.wbs_d[c0 // 128:(c0 + cw) // 128, :, rc, :].rearrange("c p n -> p c n"))
        do(k.w_out, k.wb_out, D, D); do(k.w_router, k.wb_router, D, NE)
        do(k.w_sgu, k.wb_sgu, D, 2 * FF); do(k.w_sd, k.wb_sd, FF, D)
        if pend: pend.pop()()


def rope_tables(k, st, P, t0, cosT, sinT, b_cs, tmp):
    nc = k.nc
    posi, posf, ang, kf, ki, r = tmp
    b = Buf()
    pos_bc = bass.AP(k.pos.tensor, k.pos.offset + t0, [[0, 64], [1, 512]])
    P.dma("sync", "dma_start", A(out=posi[:], in_=pos_bc), writes=[b])
    P.op("vector", "tensor_copy", A(out=posf[:], in_=posi[:]), reads=[b], writes=[b])
    P.op("vector", "tensor_scalar", A(out=ang[:], in0=posf[:], scalar1=k.cst[0:64, 256:257], scalar2=None, op0=ALU.mult),
         reads=[b, k.b_cst], writes=[b])
    for which, dst in ((0, sinT), (1, cosT)):
        src = ang
        if which == 1:
            P.op("vector", "tensor_scalar", A(out=posf[:], in0=ang[:], scalar1=math.pi / 2, scalar2=None, op0=ALU.add), reads=[b], writes=[b])
            src = posf
        P.op("vector", "tensor_scalar", A(out=kf[:], in0=src[:], scalar1=1.0 / TWO_PI, scalar2=None, op0=ALU.mult), reads=[b], writes=[b])
        P.op("vector", "tensor_copy", A(out=ki[:], in_=kf[:]), reads=[b], writes=[b])
        P.op("vector", "tensor_copy", A(out=kf[:], in_=ki[:]), reads=[b], writes=[b])
        P.op("vector", "scalar_tensor_tensor", A(out=r[:], in0=kf[:], scalar=-TWO_PI, in1=src[:], op0=ALU.mult, op1=ALU.add), reads=[b], writes=[b])
        P.op("vector", "tensor_scalar", A(out=kf[:], in0=r[:], scalar1=math.pi, scalar2=-TWO_PI, op0=ALU.is_gt, op1=ALU.mult), reads=[b], writes=[b])
        P.op("vector", "tensor_tensor", A(out=r[:], in0=r[:], in1=kf[:], op=ALU.add), reads=[b], writes=[b])
        P.op("vector", "tensor_scalar", A(out=r[:], in0=r[:], scalar1=-math.pi, scalar2=math.pi, op0=ALU.max, op1=ALU.min), reads=[b], writes=[b])
        P.op("scalar", "activation", A(out=dst[:], in_=r[:], func=AF.Sin), reads=[b], writes=[b_cs])
        b.writers = list(b_cs.writers)


def phase_A(k):
    nc, P, banks = k.nc, k.P, k.banks
    T = k.T
    with ExitStack() as st:
        def sb(name, shape, dt):
            return st.enter_context(nc.sbuf_tensor(name, list(shape), dt))
        wA = sb("wA", [128, KC, NA], BF16); wkrr = sb("wkrr", [128, KC, 64], BF16)
        wuq = sb("wuq", [128, 4, 1536], BF16); wuqr = sb("wuqr", [128, 4, 512], BF16)
        wuk = sb("wuk", [128, 2, 1024], BF16); wuv = sb("wuv", [128, 2, 1024], BF16)
        gq = sb("gq", [128, 4], F32); gkv = sb("gkv", [128, 2], F32)
        b_w = Buf()
        P.dma("sync", "dma_start", A(out=wA[:], in_=k.wb_in[:, 0:NA].rearrange("(c p) n -> p c n", p=128)), writes=[b_w])
        P.dma("sync", "dma_start", A(out=wkrr[:], in_=k.wb_krrot.rearrange("(c p) n -> p c n", p=128)), writes=[b_w])
        P.dma("sync", "dma_start", A(out=wuq[:], in_=k.wb_uq.rearrange("(c p) n -> p c n", p=128)), writes=[b_w])
        P.dma("sync", "dma_start", A(out=wuqr[:], in_=k.wb_uqrot.rearrange("(c p) n -> p c n", p=128)), writes=[b_w])
        P.dma("sync", "dma_start", A(out=wuk[:], in_=k.wb_uk.rearrange("(c p) n -> p c n", p=128)), writes=[b_w])
        P.dma("sync", "dma_start", A(out=wuv[:], in_=k.wb_uv.rearrange("(c p) n -> p c n", p=128)), writes=[b_w])
        P.dma("sync", "dma_start", A(out=gq[:], in_=k.q_norm_g.rearrange("(c p) -> p c", p=128), allow_slow_non_contiguous=True), writes=[b_w])
        P.dma("sync", "dma_start", A(out=gkv[:], in_=k.kv_norm_g.rearrange("(c p) -> p c", p=128), allow_slow_non_contiguous=True), writes=[b_w])
        xrow = [sb("xrow0", [128, D], F32)] * 2; b_xrow = [Buf()] * 2
        xb = [sb(f"xb{i}", [128, D], BF16) for i in range(2)]; b_xb = [Buf(), Buf()]
        xT = sb("xT", [128, KC, 512], BF16); b_xT = Buf()
        cqn = sb("cqn", [128, 4, 512], BF16); b_cqn = Buf()
        ckvn = sb("ckvn", [128, 2, 512], BF16); b_ckvn = Buf()
        sq = [sb(f"sq{i}", [128, 512], BF16) for i in range(4)]; b_sq = [Buf() for _ in range(4)]
        rstd = sb("rstd", [128, 512], F32); b_rstd = Buf()
        cosT = sb("cosT", [64, 512], F32); sinT = sb("sinT", [64, 512], F32); b_cs = Buf()
        tmp = (sb("posi", [64, 512], I32), sb("posf", [64, 512], F32), sb("ang", [64, 512], F32),
               sb("kf", [64, 512], F32), sb("ki", [64, 512], I32), sb("rr", [64, 512], F32))
        t1 = sb("t1", [64, 512], F32); t2 = sb("t2", [64, 512], F32); b_t = Buf()
        stgs = [sb(f"stgA{i}", [128, 8, 512], BF16) for i in range(2)]; b_stgs = [Buf(), Buf()]
        stg_i = [0]
        def next_stage():
            i = stg_i[0] % 2; stg_i[0] += 1
            return stgs[i], b_stgs[i]
        stkr = sb("stkr", [64, 512], BF16); b_stkr = Buf()
        evac_i = [0]
        def evac(out, in_, reads, writes):
            evac_i[0] += 1
            if evac_i[0] % 2 == 0:
                return P.op("scalar", "activation", A(out=out, in_=in_, func=AF.Copy), reads=reads, writes=writes)
            return P.op("vector", "tensor_copy", A(out=out, in_=in_), reads=reads, writes=writes)
        def acc(out_ap, pairs, reads, b_out):
            n = len(pairs)
            for i, (l, r) in enumerate(pairs):
                P.op("tensor", "matmul", A(out_ap, lhsT=l, rhs=r, start=(i == 0), stop=(i == n - 1)),
                     reads=reads, writes=[b_out], signal=(i == n - 1))
        for g in range(k.NG):
            t0 = g * 512
            rope_tables(k, st, P, t0, cosT, sinT, b_cs, tmp)
            for tt in range(4):
                s = tt % 2
                r0 = t0 + tt * 128
                P.dma("sync", "dma_start", A(out=xrow[s][:], in_=k.x[r0:r0 + 128, :]), writes=[b_xrow[s]])
                P.op("gpsimd", "tensor_copy", A(out=xb[s][:, 0:1024], in_=xrow[s][:, 0:1024]), reads=[b_xrow[s]], writes=[b_xb[s]])
                P.op("vector", "tensor_copy", A(out=xb[s][:, 1024:2048], in_=xrow[s][:, 1024:2048]), reads=[b_xrow[s]], writes=[b_xb[s]])
                for half in range(2):
                    bk, bb = banks.get()
                    bkb = bk[:, :].bitcast(BF16)
                    for j in range(8):
                        c = half * 8 + j
                        P.op("tensor", "transpose", A(out=bkb[:, j * 128:(j + 1) * 128], in_=xb[s][:, c * 128:(c + 1) * 128], identity=k.identb[:]),
                             reads=[b_xb[s], k.b_id], writes=[bb], signal=(j == 7))
                    evac(xT[:, half * 8:(half + 1) * 8, tt * 128:(tt + 1) * 128], bkb.rearrange("p (c t) -> p c t", t=128), [bb], [b_xT])
            P.dma("sync", "dma_start", A(out=k.xT_d[:, t0:t0 + 512].rearrange("(c p) t -> p c t", p=128), in_=xT[:]), reads=[b_xT])
            for (off, nch, gvec, dst, b_dst, nfeat) in ((OFF_CQ, 4, gq, cqn, b_cqn, 512.0), (OFF_CKV, 2, gkv, ckvn, b_ckvn, 256.0)):
                held = []
                for m in range(nch):
                    bk, bb = banks.get(hold=True)
                    acc(bk[:, :], [(wA[:, kk, off + m * 128: off + (m + 1) * 128], xT[:, kk, :]) for kk in range(KC)], [b_w, b_xT], bb)
                    P.op("scalar", "activation", A(out=sq[m][:], in_=bk[:, :], func=AF.Square), reads=[bb], writes=[b_sq[m]])
                    held.append((bk, bb))
                bs, bbs = banks.get()
                acc(bs[:, :], [(k.onesb[:], sq[m][:]) for m in range(nch)], [k.b_id] + b_sq[:nch], bbs)
                P.op("scalar", "activation", A(out=rstd[:], in_=bs[:, :], func=AF.Sqrt, scale=1.0 / nfeat, bias=k.epsr[:]), reads=[bbs, k.b_id], writes=[b_rstd])
                P.op("vector", "reciprocal", A(out=rstd[:], in_=rstd[:]), reads=[b_rstd], writes=[b_rstd])
                for m in range(nch):
                    bk, bb = held[m]
                    P.op("vector", "scalar_tensor_tensor", A(out=dst[:, m, :], in0=bk[:, :], scalar=gvec[:, m:m + 1], in1=rstd[:], op0=ALU.mult, op1=ALU.mult),
                         reads=[bb, b_rstd, b_w], writes=[b_dst])
                    banks.release(bb)
            stq, b_stq = next_stage(); str_, b_str = next_stage()
            for h in range(8):
                bk, bb = banks.get()
                acc(bk[:, :], [(wuq[:, kk, h * 192:h * 192 + 128], cqn[:, kk, :]) for kk in range(4)], [b_w, b_cqn], bb)
                evac(stq[:, h, :], bk[:, :], [bb], [b_stq])
                bA, bbA = banks.get(); bB, bbB = banks.get()
                acc(bA[0:64, :], [(wuq[:, kk, h * 192 + 128:h * 192 + 192], cqn[:, kk, :]) for kk in range(4)], [b_w, b_cqn], bbA)
                acc(bB[0:64, :], [(wuqr[:, kk, h * 64:(h + 1) * 64], cqn[:, kk, :]) for kk in range(4)], [b_w, b_cqn], bbB)
                P.op("vector", "tensor_tensor", A(out=t1[:], in0=bA[0:64, :], in1=cosT[:], op=ALU.mult), reads=[bbA, b_cs], writes=[b_t])
                P.op("vector", "tensor_tensor", A(out=t2[:], in0=bB[0:64, :], in1=sinT[:], op=ALU.mult), reads=[bbB, b_cs, b_t], writes=[b_t])
                P.op("gpsimd", "tensor_tensor", A(out=str_[0:64, h, :], in0=t1[:], in1=t2[:], op=ALU.add), reads=[b_t], writes=[b_str])
            P.dma("sync", "dma_start", A(out=k.qn_d[:, t0:t0 + 512].rearrange("(h p) t -> p h t", p=128), in_=stq[:]), reads=[b_stq])
            P.dma("sync", "dma_start", A(out=k.qr_d[:, t0:t0 + 512].rearrange("(h p) t -> p h t", p=64), in_=str_[0:64, :, :]), reads=[b_str])
            stk, b_stk = next_stage()
            for h in range(8):
                bk, bb = banks.get()
                acc(bk[:, :], [(wuk[:, kk, h * 128:(h + 1) * 128], ckvn[:, kk, :]) for kk in range(2)], [b_w, b_ckvn], bb)
                evac(stk[:, h, :], bk[:, :], [bb], [b_stk])
            P.dma("sync", "dma_start", A(out=k.kn_d[:, t0:t0 + 512].rearrange("(h p) t -> p h t", p=128), in_=stk[:]), reads=[b_stk])
            stv_, b_stv = next_stage()
            stv = stv_[:].rearrange("p a b -> p (a b)").rearrange("p (t n) -> p t n", n=1024)
            for tt in range(4):
                for half in range(2):
                    bk, bb = banks.get()
                    acc(bk[:, :], [(ckvn[:, kk, tt * 128:(tt + 1) * 128], wuv[:, kk, half * 512:(half + 1) * 512]) for kk in range(2)], [b_w, b_ckvn], bb)
                    evac(stv[:, tt, half * 512:(half + 1) * 512], bk[:, :], [bb], [b_stv])
            P.dma("sync", "dma_start", A(out=k.vm_d[t0:t0 + 512, :].rearrange("(tt p) n -> p tt n", p=128), in_=stv), reads=[b_stv])
            bA, bbA = banks.get(); bB, bbB = banks.get()
            acc(bA[0:64, :], [(wA[:, kk, OFF_KR:OFF_KR + 64], xT[:, kk, :]) for kk in range(KC)], [b_w, b_xT], bbA)
            acc(bB[0:64, :], [(wkrr[:, kk, :], xT[:, kk, :]) for kk in range(KC)], [b_w, b_xT], bbB)
            P.op("vector", "tensor_tensor", A(out=t1[:], in0=bA[0:64, :], in1=cosT[:], op=ALU.mult), reads=[bbA, b_cs], writes=[b_t])
            P.op("vector", "tensor_tensor", A(out=t2[:], in0=bB[0:64, :], in1=sinT[:], op=ALU.mult), reads=[bbB, b_cs, b_t], writes=[b_t])
            P.op("gpsimd", "tensor_tensor", A(out=stkr[:], in0=t1[:], in1=t2[:], op=ALU.add), reads=[b_t], writes=[b_stkr])
            P.dma("sync", "dma_start", A(out=k.kr_d[:, t0:t0 + 512], in_=stkr[:]), reads=[b_stkr])
            stq, b_stq = next_stage()
            for m in range(8):
                bk, bb = banks.get()
                acc(bk[:, :], [(wA[:, kk, OFF_QS + m * 128:OFF_QS + (m + 1) * 128], xT[:, kk, :]) for kk in range(KC)], [b_w, b_xT], bb)
                evac(stq[:, m, :], bk[:, :], [bb], [b_stq])
            P.dma("sync", "dma_start", A(out=k.qs_d[:, t0:t0 + 512].rearrange("(h p) t -> p h t", p=128), in_=stq[:]), reads=[b_stq])
            stk, b_stk = next_stage()
            for m in range(2):
                bk, bb = banks.get()
                acc(bk[:, :], [(wA[:, kk, OFF_KS + m * 128:OFF_KS + (m + 1) * 128], xT[:, kk, :]) for kk in range(KC)], [b_w, b_xT], bb)
                evac(stk[:, m, :], bk[:, :], [bb], [b_stk])
            P.dma("sync", "dma_start", A(out=k.ks_d[:, t0:t0 + 512].rearrange("(h p) t -> p h t", p=128), in_=stk[:, 0:2, :]), reads=[b_stk])
            sts_, b_sts = next_stage()
            sts = sts_[:].rearrange("p a b -> p (a b)")[:, 0:1024].rearrange("p (t n) -> p t n", n=256)
            for tt in range(4):
                bk, bb = banks.get()
                acc(bk[:, 0:256], [(xT[:, kk, tt * 128:(tt + 1) * 128], wA[:, kk, OFF_VS:OFF_VS + 256]) for kk in range(KC)], [b_w, b_xT], bb)
                evac(sts[:, tt, :], bk[:, 0:256], [bb], [b_sts])
            P.dma("sync", "dma_start", A(out=k.vs_d[t0:t0 + 512, :].rearrange("(tt p) n -> p tt n", p=128), in_=sts), reads=[b_sts])
        P.flush()


def phase_B(k):
    nc, P, banks = k.nc, k.P, k.banks
    S = k.S; NKT = S // 128; NQG = S // 512
    scale = 192.0 ** -0.5
    with ExitStack() as st:
        def sb(name, shape, dt):
            return st.enter_context(nc.sbuf_tensor(name, list(shape), dt))
        qn = [sb(f"b_qn{i}", [128, S], BF16) for i in range(2)]; qr = [sb(f"b_qr{i}", [64, S], BF16) for i in range(2)]
        kn = [sb(f"b_kn{i}", [128, S], BF16) for i in range(2)]; vv = [sb(f"b_v{i}", [128, NKT, 128], BF16) for i in range(2)]
        b_in = [Buf(), Buf()]
        kr = sb("b_kr", [64, S], BF16); b_kr = Buf()
        NET = 4
        et = [sb(f"b_et{i}", [128, 512], BF16) for i in range(NET)]; b_et = [Buf() for _ in range(NET)]
        rc = sb("b_rc", [128, 512], F32); b_rc = Buf()
        om = [sb(f"b_om{i}", [128, 512], BF16) for i in range(2)]; b_om = [Buf(), Buf()]
        nrows = NE * k.CAP + 128
        first = True
        for r0 in range(0, nrows, 1024):
            a = min(1024, nrows - r0) // 128
            P.dma("gpsimd", "dma_start", A(out=k.xg_d[r0:r0 + a * 128, :].rearrange("(a p) d -> p a d", p=128), in_=k.zrowb[:].unsqueeze(1).broadcast_to([128, a, D])),
                  reads=[k.b_z], **(dict(writes=[k.b_xg]) if first else dict(joins=[k.b_xg])))
            first = False
        P.dma("gpsimd", "dma_start", A(out=k.yg_d[NE * k.CAP:NE * k.CAP + 128, :], in_=k.zrowb[:]), reads=[k.b_z], writes=[k.b_yg])
        phase_W(k, 2, st)
        it = 0; ei = 0; oi = 0
        for sq_ in range(k.NSEQ):
            tb = sq_ * S
            P.dma("sync", "dma_start", A(out=kr[:], in_=k.kr_d[:, tb:tb + S]), writes=[b_kr])
            for h in range(8):
                i = it % 2; it += 1
                P.dma("sync", "dma_start", A(out=qn[i][:], in_=k.qn_d[h * 128:(h + 1) * 128, tb:tb + S]), writes=[b_in[i]])
                P.dma("sync", "dma_start", A(out=qr[i][:], in_=k.qr_d[h * 64:(h + 1) * 64, tb:tb + S]), joins=[b_in[i]])
                P.dma("sync", "dma_start", A(out=kn[i][:], in_=k.kn_d[h * 128:(h + 1) * 128, tb:tb + S]), joins=[b_in[i]])
                P.dma("sync", "dma_start", A(out=vv[i][:], in_=k.vm_d[tb:tb + S, h * 128:(h + 1) * 128].rearrange("(j p) n -> p j n", p=128)), joins=[b_in[i]])
                for G in range(NQG):
                    q0 = G * 512
                    bo, bbo = banks.get(hold=True); bsum, bbs = banks.get(hold=True)
                    nkt = 4 * G + 4
                    def c0_of(j):
                        r = j - 4 * G
                        return 128 * r if r > 0 else 0
                    staged = {}
                    def stage1(j):
                        nonlocal ei
                        c0 = c0_of(j)
                        bs_, bbs_ = banks.get()
                        P.op("tensor", "matmul", A(bs_[:, c0:512], lhsT=kn[i][:, j * 128:(j + 1) * 128], rhs=qn[i][:, q0 + c0:q0 + 512], start=True, stop=False),
                             reads=[b_in[i]], writes=[bbs_], signal=False)
                        P.op("tensor", "matmul", A(bs_[:, c0:512], lhsT=kr[:, j * 128:(j + 1) * 128], rhs=qr[i][:, q0 + c0:q0 + 512], start=False, stop=True),
                             reads=[b_in[i], b_kr], writes=[bbs_])
                        e = ei % NET; ei += 1
                        P.op("scalar", "activation", A(out=et[e][:, c0:512], in_=bs_[:, c0:512], func=AF.Exp, scale=scale), reads=[bbs_], writes=[b_et[e]])
                        if j - 4 * G >= 0:
                            P.op("vector", "tensor_tensor", A(out=et[e][:, c0:c0 + 128], in0=et[e][:, c0:c0 + 128], in1=k.trib[:], op=ALU.mult),
                                 reads=[k.b_id], writes=[b_et[e]])
                        staged[j] = e
                    def stage2(j):
                        c0 = c0_of(j); e = staged.pop(j)
                        first = (j == 0); last = (j == nkt - 1)
                        P.op("tensor", "matmul", A(bo[:, c0:512], lhsT=vv[i][:, j, :], rhs=et[e][:, c0:512], start=first, stop=last),
                             reads=[b_in[i], b_et[e]], writes=[bbo], signal=False)
                        P.op("tensor", "matmul", A(bsum[:, c0:512], lhsT=k.onesb[:], rhs=et[e][:, c0:512], start=first, stop=last),
                             reads=[k.b_id, b_et[e]], writes=[bbs], signal=True)
                    LOOK = 2
                    for j in range(min(LOOK, nkt)): stage1(j)
                    for j in range(nkt):
                        if j + LOOK < nkt: stage1(j + LOOK)
                        stage2(j)
                    P.op("vector", "reciprocal", A(out=rc[:], in_=bsum[:, :]), reads=[bbs], writes=[b_rc])
                    o = oi % 2; oi += 1
                    P.op("vector", "tensor_tensor", A(out=om[o][:], in0=bo[:, :], in1=rc[:], op=ALU.mult), reads=[bbo, b_rc], writes=[b_om[o]])
                    P.dma("sync", "dma_start", A(out=k.omT_d[h * 128:(h + 1) * 128, tb + q0:tb + q0 + 512], in_=om[o][:]), reads=[b_om[o]])
                    banks.release(bbo); banks.release(bbs)
        P.flush()


def phase_C(k):
    nc, P, banks = k.nc, k.P, k.banks
    S = k.S; NKT = S // 128
    scale = 64.0 ** -0.5
    with ExitStack() as st:
        def sb(name, shape, dt):
            return st.enter_context(nc.sbuf_tensor(name, list(shape), dt))
        rt = sb("c_rt", [32, 16], F32); rtb = sb("c_rtb", [32, 16], BF16); ohb = sb("c_ohb", [32, 128], BF16)
        tp = sb("c_tp", [16, 512], F32); b_s = Buf()
        biasC = sb("c_biasC", [128, 16, 128], F32); biasP = sb("c_biasP", [128, 16, 128], F32); b_bias = Buf()
        sk = sb("c_sk", [64, 16], F32); b_sk = Buf()
        biasCb = sb("c_biasCb", [128, 16, 128], BF16); biasPb = sb("c_biasPb", [128, 16, 128], BF16); b_biasb = Buf()
        P.dma("sync", "dma_start", A(out=rt[:], in_=k.rel_table), writes=[b_s])
        P.op("vector", "tensor_copy", A(out=rtb[:], in_=rt[:]), reads=[b_s], writes=[b_s])
        P.op("vector", "tensor_copy", A(out=ohb[:], in_=k.cst[0:32, 257:385]), reads=[k.b_cst], writes=[b_s])
        P.op("vector", "memset", A(tp[:], NEG), writes=[b_s])
        bk, bb = banks.get()
        P.op("tensor", "matmul", A(bk[0:16, 0:128], lhsT=rtb[:], rhs=ohb[:], start=True, stop=True), reads=[b_s], writes=[bb])
        P.op("vector", "tensor_copy", A(out=tp[:, 128:256], in_=bk[0:16, 0:128]), reads=[bb, b_s], writes=[b_s])
        tw = P.dma("sync", "dma_start", A(out=k.tpad_d, in_=tp[:]), reads=[b_s])
        trep = sb("c_trep", [128, 16, 512], F32); b_tr = Buf()
        P.dma("sync", "dma_start", A(out=trep[:], in_=bass.AP(k.tpad_d.tensor, k.tpad_d.offset, [[0, 128], [512, 16], [1, 512]])), writes=[b_tr], deps=[tw])
        ZS = 128 * 513
        for h in range(16):
            zw = P.dma("sync", "dma_start", A(out=bass.AP(k.zf_d.tensor, k.zf_d.offset + h * ZS, [[513, 128], [1, 512]]), in_=trep[:, h, :]), reads=[b_tr])
            apC = bass.AP(k.zf_d.tensor, k.zf_d.offset + h * ZS + 128, [[512, 128], [1, 128]])
            apP = bass.AP(k.zf_d.tensor, k.zf_d.offset + h * ZS + 256, [[512, 128], [1, 128]])
            P.dma("sync", "dma_start", A(out=biasC[:, h, :], in_=apC), writes=[b_bias], deps=[zw])
            P.dma("sync", "dma_start", A(out=biasP[:, h, :], in_=apP), writes=[b_bias], deps=[zw])
        P.op("vector", "tensor_scalar", A(out=biasCb[:], in0=biasC[:], scalar1=1.0 / scale, scalar2=None, op0=ALU.mult), reads=[b_bias], writes=[b_biasb])
        P.op("vector", "tensor_scalar", A(out=biasPb[:], in0=biasP[:], scalar1=1.0 / scale, scalar2=None, op0=ALU.mult), reads=[b_bias], joins=[b_biasb])
        sink_bc = bass.AP(k.sinks.tensor, k.sinks.offset, [[0, 64], [1, 16]])
        P.dma("sync", "dma_start", A(out=sk[:], in_=sink_bc), writes=[b_sk])
        P.op("scalar", "activation", A(out=sk[:], in_=sk[:], func=AF.Exp), reads=[b_sk], writes=[b_sk])
        qs = [sb(f"c_qs{i}", [64, 4, S], BF16) for i in range(2)]; ks = [sb(f"c_ks{i}", [64, S], BF16) for i in range(2)]
        vs = [sb(f"c_vs{i}", [128, NKT, 64], BF16) for i in range(2)]; b_in = [Buf(), Buf()]
        rc = sb("c_rc", [64, 4, 128], F32); b_rc = Buf()
        osb = [sb(f"c_os{i}", [64, 4, 128], BF16) for i in range(2)]; b_os = [Buf(), Buf()]
        NETC = 8; NTMP = 6
        et = [sb(f"c_et{i}", [128, 512], BF16) for i in range(NETC)]; b_et = [Buf() for _ in range(NETC)]
        it = 0; cnt = {"t": 0, "e": 0, "o": 0}
        iters = []
        for sq_ in range(k.NSEQ):
            for g in range(4):
                for qi in range(NKT):
                    iters.append((sq_, g, qi))
        loaded = {}
        def ensure_loaded(sq_, g):
            nonlocal it
            if (sq_, g) in loaded: return loaded[(sq_, g)]
            i = it % 2; it += 1
            tb = sq_ * S
            P.dma("sync", "dma_start", A(out=qs[i][:], in_=k.qs_d[g * 256:(g + 1) * 256, tb:tb + S].rearrange("(h p) t -> p h t", p=64)), writes=[b_in[i]])
            P.dma("sync", "dma_start", A(out=ks[i][:], in_=k.ks_d[g * 64:(g + 1) * 64, tb:tb + S]), joins=[b_in[i]])
            P.dma("sync", "dma_start", A(out=vs[i][:], in_=k.vs_d[tb:tb + S, g * 64:(g + 1) * 64].rearrange("(j p) n -> p j n", p=128)), joins=[b_in[i]])
            loaded[(sq_, g)] = i
            return i
        staged = {}
        def stage1(n):
            sq_, g, qi = iters[n]
            i = ensure_loaded(sq_, g)
            blocks = ([(qi - 1, biasPb)] if qi > 0 else []) + [(qi, biasCb)]
            res = []
            for (kj, bias) in blocks:
                bs_, bbs_ = banks.get()
                P.op("tensor", "matmul", A(bs_[:, :].rearrange("p (h q) -> p h q", q=128), lhsT=ks[i][:, kj * 128:(kj + 1) * 128], rhs=qs[i][:, :, qi * 128:(qi + 1) * 128], start=True, stop=False),
                     reads=[b_in[i]], writes=[bbs_], signal=False)
                P.op("tensor", "matmul", A(bs_[:, :].rearrange("p (h q) -> p h q", q=128), lhsT=k.identb[:], rhs=bias[:, 4 * g:4 * g + 4, :], start=False, stop=True),
                     reads=[k.b_id, b_biasb], writes=[bbs_])
                e = cnt["e"] % NETC; cnt["e"] += 1
                P.op("scalar", "activation", A(out=et[e][:], in_=bs_[:, :], func=AF.Exp, scale=scale), reads=[bbs_], writes=[b_et[e]])
                res.append((kj, e))
            staged[n] = (i, res)
        def stage2(n):
            sq_, g, qi = iters[n]
            tb = sq_ * S
            i, res = staged.pop(n)
            bo, bbo = banks.get(hold=True); bsum, bbs = banks.get(hold=True)
            for bi, (kj, e) in enumerate(res):
                first = (bi == 0); last = (bi == len(res) - 1)
                P.op("tensor", "matmul", A(bo[0:64, :], lhsT=vs[i][:, kj, :], rhs=et[e][:], start=first, stop=last), reads=[b_in[i], b_et[e]], writes=[bbo], signal=False)
                P.op("tensor", "matmul", A(bsum[0:64, :], lhsT=k.onesb[:, 0:64], rhs=et[e][:], start=first, stop=last), reads=[k.b_id, b_et[e]], writes=[bbs], signal=True)
            P.op("vector", "tensor_tensor", A(out=rc[:], in0=bsum[0:64, :].rearrange("p (h q) -> p h q", q=128), in1=sk[:, 4 * g:4 * g + 4].unsqueeze(2).broadcast_to([64, 4, 128]), op=ALU.add),
                 reads=[bbs, b_sk], writes=[b_rc])
            P.op("vector", "reciprocal", A(out=rc[:], in_=rc[:]), reads=[b_rc], writes=[b_rc])
            o = cnt["o"] % 2; cnt["o"] += 1
            P.op("vector", "tensor_tensor", A(out=osb[o][:], in0=bo[0:64, :].rearrange("p (h q) -> p h q", q=128), in1=rc[:], op=ALU.mult), reads=[bbo, b_rc], writes=[b_os[o]])
            P.dma("sync", "dma_start", A(out=k.osT_d[g * 256:(g + 1) * 256, tb + qi * 128:tb + (qi + 1) * 128].rearrange("(h p) t -> p h t", p=64), in_=osb[o][:]), reads=[b_os[o]])
            banks.release(bbo); banks.release(bbs)
        LOOK = 2
        for n in range(min(LOOK, len(iters))): stage1(n)
        for n in range(len(iters)):
            if n + LOOK < len(iters): stage1(n + LOOK)
            stage2(n)
        P.flush()


def phase_D(k):
    nc, P, banks = k.nc, k.P, k.banks
    with ExitStack() as st:
        def sb(name, shape, dt):
            return st.enter_context(nc.sbuf_tensor(name, list(shape), dt))
        xT = sb("d_xT", [128, KC, 512], BF16); om = sb("d_om", [128, 8, 512], BF16); osb = sb("d_os", [128, 8, 512], BF16); b_act = Buf()
        bg = sb("d_bg", [128, 32], F32); b_bg = Buf()
        P.dma("sync", "dma_start", A(out=bg[:], in_=k.b_gate.rearrange("(c p) -> p c", p=128), allow_slow_non_contiguous=True), writes=[b_bg])
        wg = [sb(f"d_wg{i}", [128, 2, KC, 128], BF16) for i in range(2)]
        wb = [sb(f"d_wb{i}", [128, 2, 8, 128], BF16) for i in range(2)]; b_w = [Buf(), Buf()]
        s0 = sb("d_s0", [128, 512], F32); s1 = sb("d_s1", [128, 512], F32); b_s0 = Buf(); b_s1 = Buf()
        m1 = sb("d_m1", [128, 512], F32); m2 = sb("d_m2", [128, 512], F32); b_m1 = Buf(); b_m2 = Buf()
        mT = sb("d_mT", [128, KC, 512], BF16); b_mT = Buf()
        wi = 0
        for g in range(k.NG):
            t0 = g * 512
            P.dma("sync", "dma_start", A(out=xT[:], in_=k.xT_d[:, t0:t0 + 512].rearrange("(c p) t -> p c t", p=128)), writes=[b_act])
            P.dma("sync", "dma_start", A(out=om[:], in_=k.omT_d[:, t0:t0 + 512].rearrange("(c p) t -> p c t", p=128)), writes=[b_act])
            P.dma("sync", "dma_start", A(out=osb[:], in_=k.osT_d[:, t0:t0 + 512].rearrange("(c p) t -> p c t", p=128)), writes=[b_act])
            for c in range(KC):
                i = wi % 2; wi += 1
                P.dma("sync", "dma_start", A(out=wg[i][:, 0], in_=k.wg_d[c]), writes=[b_w[i]])
                P.dma("sync", "dma_start", A(out=wg[i][:, 1], in_=k.wg_d[KC + c]), writes=[b_w[i]])
                P.dma("sync", "dma_start", A(out=wb[i][:, 0], in_=k.wbm_d[c]), writes=[b_w[i]])
                P.dma("sync", "dma_start", A(out=wb[i][:, 1], in_=k.wbs_d[c]), writes=[b_w[i]])
                bym, bbym = banks.get(); bys, bbys = banks.get(); bg0, bbg0 = banks.get(); bg1, bbg1 = banks.get()
                def acc(out_ap, pairs, reads, b_out):
                    n = len(pairs)
                    for ii, (l, r) in enumerate(pairs):
                        P.op("tensor", "matmul", A(out_ap, lhsT=l, rhs=r, start=(ii == 0), stop=(ii == n - 1)), reads=reads, writes=[b_out], signal=(ii == n - 1))
                acc(bg0[:, :], [(wg[i][:, 0, kk, :], xT[:, kk, :]) for kk in range(KC)], [b_w[i], b_act], bbg0)
                acc(bg1[:, :], [(wg[i][:, 1, kk, :], xT[:, kk, :]) for kk in range(KC)], [b_w[i], b_act], bbg1)
                acc(bym[:, :], [(wb[i][:, 0, kk, :], om[:, kk, :]) for kk in range(8)], [b_w[i], b_act], bbym)
                acc(bys[:, :], [(wb[i][:, 1, kk, :], osb[:, kk, :]) for kk in range(8)], [b_w[i], b_act], bbys)
                P.op("scalar", "activation", A(out=s0[:], in_=bg0[:, :], func=AF.Sigmoid, bias=bg[:, c:c + 1]), reads=[bbg0, b_bg], writes=[b_s0])
                P.op("scalar", "activation", A(out=s1[:], in_=bg1[:, :], func=AF.Sigmoid, bias=bg[:, KC + c:KC + c + 1]), reads=[bbg1, b_bg], writes=[b_s1])
                P.op("vector", "tensor_tensor", A(out=m1[:], in0=bym[:, :], in1=s0[:], op=ALU.mult), reads=[bbym, b_s0], writes=[b_m1])
                P.op("vector", "tensor_tensor", A(out=m2[:], in0=bys[:, :], in1=s1[:], op=ALU.mult), reads=[bbys, b_s1], writes=[b_m2])
                P.op("gpsimd", "tensor_tensor", A(out=mT[:, c, :], in0=m1[:], in1=m2[:], op=ALU.add), reads=[b_m1, b_m2], writes=[b_mT])
            P.dma("sync", "dma_start", A(out=k.mT_d[:, t0:t0 + 512].rearrange("(c p) t -> p c t", p=128), in_=mT[:]), reads=[b_mT])
        P.flush()


def layer_norm_rows(P, k, pre, b_pre, out, b_out, g_t, b_t, b_gb, stats, mv, b_st, eng2="gpsimd"):
    for c in range(4):
        P.op("vector", "bn_stats", A(out=stats[:, c, :], in_=pre[:, c * 512:(c + 1) * 512]), reads=[b_pre], writes=[b_st])
    P.op("vector", "bn_aggr", A(out=mv[:, 0:2], in_=stats[:].rearrange("p c s -> p (c s)")), reads=[b_st], writes=[b_st])
    P.op("scalar", "activation", A(out=mv[:, 2:3], in_=mv[:, 1:2], func=AF.Sqrt, bias=k.epsl[:]), reads=[b_st, k.b_id], writes=[b_st])
    P.op("vector", "reciprocal", A(out=mv[:, 2:3], in_=mv[:, 2:3]), reads=[b_st], writes=[b_st])
    P.op("vector", "tensor_scalar", A(out=pre[:], in0=pre[:], scalar1=mv[:, 0:1], scalar2=mv[:, 2:3], op0=ALU.subtract, op1=ALU.mult), reads=[b_st], writes=[b_pre])
    P.op(eng2, "tensor_tensor", A(out=pre[:], in0=pre[:], in1=g_t[:], op=ALU.mult), reads=[b_gb], writes=[b_pre])
    P.op(eng2, "tensor_tensor", A(out=out[:], in0=pre[:], in1=b_t[:], op=ALU.add), reads=[b_gb, b_pre], writes=[b_out])


def phase_E(k):
    nc, P, banks = k.nc, k.P, k.banks
    with ExitStack() as st:
        def sb(name, shape, dt):
            return st.enter_context(nc.sbuf_tensor(name, list(shape), dt))
        wout = sb("e_wout", [128, KC, D], BF16); wr = sb("e_wr", [128, KC, NE], BF16); b_w = Buf()
        P.dma("sync", "dma_start", A(out=wout[:], in_=k.wb_out.rearrange("(c p) n -> p c n", p=128)), writes=[b_w])
        P.dma("sync", "dma_start", A(out=wr[:], in_=k.wb_router.rearrange("(c p) n -> p c n", p=128)), writes=[b_w])
        g_t = sb("e_g", [128, D], F32); b_t = sb("e_b", [128, D], F32); rb = sb("e_rb", [128, NE], F32); b_gb = Buf()
        for dst, src, n in ((g_t, k.ln1_g, D), (b_t, k.ln1_b, D), (rb, k.router_bias, NE)):
            P.dma("sync", "dma_start", A(out=dst[:], in_=bass.AP(src.tensor, src.offset, [[0, 128], [1, n]])), writes=[b_gb])
        mT = sb("e_mT", [128, KC, 512], BF16); b_mT = Buf()
        xrows = [sb(f"e_x{i}", [128, D], F32) for i in range(2)]; b_xs = [Buf(), Buf()]
        pres = [sb(f"e_pre{i}", [128, D], F32) for i in range(2)]; b_pres = [Buf(), Buf()]
        hrow = [sb(f"e_h{i}", [128, D], F32) for i in range(2)]; b_h = [Buf(), Buf()]
        hb = [sb(f"e_hb{i}", [128, D], BF16) for i in range(2)]; b_hb = [Buf(), Buf()]
        hT = sb("e_hT", [128, KC, 512], BF16); b_hT = Buf()
        stats = sb("e_stats", [128, 4, 6], F32); mv = sb("e_mv", [128, 4], F32); b_st = Buf()
        sc = sb("e_sc", [128, NE], F32); sel = sb("e_sel", [128, NE], F32); r1 = sb("e_r1", [128, NE], F32); r2 = sb("e_r2", [128, NE], F32)
        g1 = sb("e_g1", [128, 8], F32); g2 = sb("e_g2", [128, 8], F32); t8 = sb("e_t8", [128, 8], F32); gm = sb("e_gm", [128, 8], F32)
        ws = sb("e_ws", [128, 2], F32); b_r = Buf()
        hi = 0
        for g in range(k.NG):
            P.dma("sync", "dma_start", A(out=mT[:], in_=k.mT_d[:, g * 512:(g + 1) * 512].rearrange("(c p) t -> p c t", p=128)), writes=[b_mT])
            for tt in range(4):
                t = g * 4 + tt; r0 = t * 128
                xrow = xrows[t % 2]; b_x = b_xs[t % 2]; pre = pres[t % 2]; b_pre = b_pres[t % 2]
                P.dma("sync", "dma_start", A(out=xrow[:], in_=k.x[r0:r0 + 128, :]), writes=[b_x])
                for n in range(4):
                    bk, bb = banks.get()
                    for kk in range(KC):
                        P.op("tensor", "matmul", A(bk[:, :], lhsT=mT[:, kk, tt * 128:(tt + 1) * 128], rhs=wout[:, kk, n * 512:(n + 1) * 512], start=(kk == 0), stop=(kk == KC - 1)),
                             reads=[b_mT, b_w], writes=[bb], signal=(kk == KC - 1))
                    P.op("vector", "scalar_tensor_tensor", A(out=pre[:, n * 512:(n + 1) * 512], in0=xrow[:, n * 512:(n + 1) * 512], scalar=ALPHA, in1=bk[:, :], op0=ALU.mult, op1=ALU.add),
                         reads=[b_x, bb], writes=[b_pre])
                i = hi % 2; hi += 1
                layer_norm_rows(P, k, pre, b_pre, hrow[i], b_h[i], g_t, b_t, b_gb, stats, mv, b_st)
                P.dma("sync", "dma_start", A(out=k.h_d[r0:r0 + 128, :], in_=hrow[i][:]), reads=[b_h[i]])
                P.op("scalar", "activation", A(out=hb[i][:], in_=hrow[i][:], func=AF.Copy), reads=[b_h[i]], writes=[b_hb[i]])
                P.dma("sync", "dma_start", A(out=k.hb_d[r0:r0 + 128, :], in_=hb[i][:]), reads=[b_hb[i]])
                for half in range(2):
                    bk, bb = banks.get()
                    bkb = bk[:, :].bitcast(BF16)
                    for j in range(8):
                        c = half * 8 + j
                        P.op("tensor", "transpose", A(out=bkb[:, j * 128:(j + 1) * 128], in_=hb[i][:, c * 128:(c + 1) * 128], identity=k.identb[:]),
                             reads=[b_hb[i], k.b_id], writes=[bb], signal=(j == 7))
                    P.op("vector", "tensor_copy", A(out=hT[:, half * 8:(half + 1) * 8, tt * 128:(tt + 1) * 128], in_=bkb.rearrange("p (c t) -> p c t", t=128)), reads=[bb], writes=[b_hT])
                bk, bb = banks.get()
                for kk in range(KC):
                    P.op("tensor", "matmul", A(bk[:, 0:NE], lhsT=hT[:, kk, tt * 128:(tt + 1) * 128], rhs=wr[:, kk, :], start=(kk == 0), stop=(kk == KC - 1)),
                         reads=[b_hT, b_w], writes=[bb], signal=(kk == KC - 1))
                P.op("scalar", "activation", A(out=sc[:], in_=bk[:, 0:NE], func=AF.Sigmoid), reads=[bb], writes=[b_r])
                V = lambda name, a, **kw: P.op("vector", name, a, reads=[b_r] + kw.get("reads", []), writes=[b_r] + kw.get("writes", []))
                sel3 = sel[:].rearrange("p (g e) -> p g e", e=8); r13 = r1[:].rearrange("p (g e) -> p g e", e=8); r23 = r2[:].rearrange("p (g e) -> p g e", e=8)
                V("tensor_tensor", A(out=sel[:], in0=sc[:], in1=rb[:], op=ALU.add), reads=[b_gb])
                V("tensor_reduce", A(out=g1[:], in_=sel3, axis=AX.X, op=ALU.max))
                V("tensor_tensor", A(out=r13, in0=sel3, in1=g1[:].unsqueeze(2).broadcast_to([128, 8, 8]), op=ALU.is_equal))
                V("scalar_tensor_tensor", A(out=r2[:], in0=r1[:], scalar=-1.0e9, in1=sel[:], op0=ALU.mult, op1=ALU.add))
                V("tensor_reduce", A(out=g2[:], in_=r23, axis=AX.X, op=ALU.max))
                V("tensor_tensor", A(out=g1[:], in0=g1[:], in1=g2[:], op=ALU.add))
                V("max", A(out=t8[:], in_=g1[:]))
                V("tensor_scalar", A(out=gm[:], in0=g1[:], scalar1=t8[:, 3:4], scalar2=None, op0=ALU.is_ge))
                V("scalar_tensor_tensor", A(out=r13, in0=sel3, scalar=2.0, in1=gm[:].unsqueeze(2).broadcast_to([128, 8, 8]), op0=ALU.add, op1=ALU.mult))
                V("max", A(out=t8[:], in_=r1[:]))
                V("tensor_scalar", A(out=k.Msk[:, t, :], in0=r1[:], scalar1=t8[:, 7:8], scalar2=None, op0=ALU.is_ge), writes=[k.b_Msk])
                V("tensor_tensor", A(out=r2[:], in0=sc[:], in1=k.Msk[:, t, :], op=ALU.mult))
                V("tensor_reduce", A(out=ws[:, 0:1], in_=r2[:], axis=AX.X, op=ALU.add))
                V("reciprocal", A(out=ws[:, 1:2], in_=ws[:, 0:1]))
                V("tensor_scalar", A(out=k.Wc[:, t, :], in0=r2[:], scalar1=ws[:, 1:2], scalar2=2.5, op0=ALU.mult, op1=ALU.mult), writes=[k.b_Wc])
            P.dma("sync", "dma_start", A(out=k.hT_d[:, g * 512:(g + 1) * 512].rearrange("(c p) t -> p c t", p=128), in_=hT[:]), reads=[b_hT])
        if "dbg_route" in k.debug:
            P.dma("sync", "dma_start", A(out=k.dbg_route[:, 0:k.NT * NE], in_=k.Wc[:].rearrange("p t e -> p (t e)")), reads=[k.b_Wc])
        P.flush()


def phase_FG(k):
    nc, P, banks = k.nc, k.P, k.banks
    NT, CAP = k.NT, k.CAP
    NC_ = NT * NE
    with ExitStack() as st:
        def sb(name, shape, dt):
            return st.enter_context(nc.sbuf_tensor(name, list(shape), dt))
        Mb = sb("f_Mb", [128, NC_], BF16); us = sb("f_us", [128, 128], BF16)
        pos = sb("f_pos", [128, NT, NE], F32); tot = sb("f_tot", [128, NT, NE], F32); base = sb("f_base", [128, NT, NE], F32)
        key = sb("f_key", [128, NT, NE], F32); k8 = sb("f_k8", [128, NT, 8], F32); iz = sb("f_iz", [128, NT, 8], F32)
        ecapi = sb("f_ecapi", [128, NE], I32); ecap = sb("f_ecap", [128, NE], F32); dump1 = sb("f_dump1", [128, 1], F32)
        junk = sb("f_junk", [128, NE], F32)
        b = Buf()
        V = lambda name, a, **kw: P.op("vector", name, a, reads=[b] + kw.get("reads", []), writes=[b] + kw.get("writes", []))
        V("tensor_copy", A(out=Mb[:], in_=k.Msk[:].rearrange("p t e -> p (t e)")), reads=[k.b_Msk])
        V("tensor_tensor", A(out=us[:], in0=k.trib[:], in1=k.identb[:], op=ALU.subtract), reads=[k.b_id])
        posf = pos[:].rearrange("p t e -> p (t e)"); totf = tot[:].rearrange("p t e -> p (t e)")
        for c0 in range(0, NC_, 512):
            cw = min(512, NC_ - c0)
            bk, bb = banks.get()
            P.op("tensor", "matmul", A(bk[:, 0:cw], lhsT=us[:], rhs=Mb[:, c0:c0 + cw], start=True, stop=True), reads=[b], writes=[bb])
            P.op("vector", "tensor_copy", A(out=posf[:, c0:c0 + cw], in_=bk[:, 0:cw]), reads=[bb, b], writes=[b])
            bk, bb = banks.get()
            P.op("tensor", "matmul", A(bk[:, 0:cw], lhsT=k.onesb[:], rhs=Mb[:, c0:c0 + cw], start=True, stop=True), reads=[b, k.b_id], writes=[bb])
            P.op("vector", "tensor_copy", A(out=totf[:, c0:c0 + cw], in_=bk[:, 0:cw]), reads=[bb, b], writes=[b])
        V("memset", A(base[:, 0, :], 0.0))
        for t in range(1, NT):
            V("tensor_tensor", A(out=base[:, t, :], in0=base[:, t - 1, :], in1=tot[:, t - 1, :], op=ALU.add))
        V("tensor_tensor", A(out=pos[:], in0=pos[:], in1=base[:], op=ALU.add))
        V("scalar_tensor_tensor", A(out=tot[:], in0=pos[:], scalar=float(CAP), in1=k.Msk[:], op0=ALU.is_lt, op1=ALU.mult), reads=[k.b_Msk])
        P.op("gpsimd", "iota", A(ecapi[:], pattern=[[CAP, NE]], base=1, channel_multiplier=0), reads=[b], writes=[b])
        V("tensor_copy", A(out=ecap[:], in_=ecapi[:]))
        V("tensor_tensor", A(out=key[:], in0=pos[:], in1=ecap[:].unsqueeze(1).broadcast_to([128, NT, NE]), op=ALU.add))
        V("tensor_tensor", A(out=key[:], in0=key[:], in1=tot[:], op=ALU.mult))
        for t in range(NT):
            V("max", A(out=k8[:, t, :], in_=key[:, t, :]))
        V("tensor_scalar", A(out=dump1[:], in0=k.cst[:, 385:386], scalar1=float(NE * CAP + 1), scalar2=None, op0=ALU.add), reads=[k.b_cst])
        V("tensor_scalar", A(out=iz[:], in0=k8[:], scalar1=0.0, scalar2=None, op0=ALU.is_equal))
        V("scalar_tensor_tensor", A(out=iz[:], in0=iz[:], scalar=dump1[:, 0:1], in1=k8[:], op0=ALU.mult, op1=ALU.add))
        V("tensor_scalar", A(out=iz[:], in0=iz[:], scalar1=-1.0, scalar2=None, op0=ALU.add))
        V("tensor_copy", A(out=k.idx[:], in_=iz[:]), writes=[k.b_idx])
        V("memset", A(k.wj[:], 0.0), writes=[k.b_wj])
        for t in range(NT):
            for j in range(8):
                V("scalar_tensor_tensor", A(out=junk[:], in0=key[:, t, :], scalar=k8[:, t, j:j + 1], in1=k.Wc[:, t, :], op0=ALU.is_equal, op1=ALU.mult, accum_out=k.wj[:, t, j:j + 1]),
                  reads=[k.b_Wc], writes=[k.b_wj])
        if "dbg_route" in k.debug:
            P.dma("sync", "dma_start", A(out=k.dbg_route[:, NT * NE:NT * NE + NT * 8], in_=iz[:].rearrange("p t j -> p (t j)")), reads=[b])
            P.dma("sync", "dma_start", A(out=k.dbg_route[:, NT * NE + NT * 8:NT * NE + NT * 16], in_=k.wj[:].rearrange("p t j -> p (t j)")), reads=[k.b_wj])
        hbr = [sb(f"f_hb{i}", [128, D], BF16) for i in range(2)]; b_hb = [Buf(), Buf()]
        first_sc = [True]
        def scatter_tile(t):
            i = t % 2
            P.dma("sync", "dma_start", A(out=hbr[i][:], in_=k.hb_d[t * 128:(t + 1) * 128, :]), writes=[b_hb[i]])
            for j in range(8):
                kw = dict(writes=[k.b_xg]) if first_sc[0] else dict(joins=[k.b_xg])
                first_sc[0] = False
                P.dma("gpsimd", "indirect_dma_start", A(out=k.xg_d, out_offset=bass.IndirectOffsetOnAxis(ap=k.idx[:, t, j:j + 1], axis=0), in_=hbr[i][:], in_offset=None),
                      reads=[b_hb[i], k.b_idx], **kw)
        wgu = sb("g_wgu", [128, KC, 1024], BF16); wd = sb("g_wd", [128, 4, D], BF16); b_w = Buf()
        P.dma("sync", "dma_start", A(out=wgu[:], in_=k.wb_sgu.rearrange("(c p) n -> p c n", p=128)), writes=[b_w])
        P.dma("sync", "dma_start", A(out=wd[:], in_=k.wb_sd.rearrange("(c p) n -> p c n", p=128)), writes=[b_w])
        hT = [sb(f"g_hT{i}", [128, KC, 512], BF16) for i in range(2)]; b_hT = [Buf(), Buf()]
        aT = sb("g_aT", [128, 4, 512], BF16); b_aT = Buf()
        tmp = [sb(f"g_tmp{i}", [128, 512], F32) for i in range(2)]; b_tmp = [Buf(), Buf()]
        yrow = [sb(f"g_y{i}", [128, D], F32) for i in range(2)]; b_y = [Buf(), Buf()]
        yi = 0
        for g in range(k.NG):
            i = g % 2
            P.dma("sync", "dma_start", A(out=hT[i][:], in_=k.hT_d[:, g * 512:(g + 1) * 512].rearrange("(c p) t -> p c t", p=128)), writes=[b_hT[i]])
            ffn_T(P, banks, wgu, b_w, lambda kk, n0, nw, i=i: hT[i][:, kk, n0:n0 + nw], b_hT[i], [(0, 512)], aT, b_aT, tmp, b_tmp, KC)
            for tt in range(4):
                y = yi % 2; yi += 1
                for n in range(4):
                    bk, bb = banks.get()
                    for c in range(4):
                        P.op("tensor", "matmul", A(bk[:, :], lhsT=aT[:, c, tt * 128:(tt + 1) * 128], rhs=wd[:, c, n * 512:(n + 1) * 512], start=(c == 0), stop=(c == 3)),
                             reads=[b_aT, b_w], writes=[bb], signal=(c == 3))
                    eng, nm, a = (("scalar", "activation", A(out=yrow[y][:, n * 512:(n + 1) * 512], in_=bk[:, :], func=AF.Copy)) if n % 2 == 0 else
                                  ("vector", "tensor_copy", A(out=yrow[y][:, n * 512:(n + 1) * 512], in_=bk[:, :])))
                    P.op(eng, nm, a, reads=[bb], writes=[b_y[y]])
                r0 = (g * 4 + tt) * 128
                P.dma("sync", "dma_start", A(out=k.ysh_d[r0:r0 + 128, :], in_=yrow[y][:]), reads=[b_y[y]])
                scatter_tile(g * 4 + tt)
        P.flush()


def ffn_T(P, banks, wgu, b_wgu, xT_ap, b_x, pieces, aT, b_aT, tmp, b_tmp, KCH):
    ti = 0
    for c in range(4):
        gb = [banks.get(hold=True) for _ in pieces]
        ub = [banks.get(hold=True) for _ in pieces]
        for (bl, col0) in ((gb, c * 128), (ub, 512 + c * 128)):
            for kk in range(KCH):
                for pi, (n0, nw) in enumerate(pieces):
                    bk_, bb_ = bl[pi]
                    P.op("tensor", "matmul", A(bk_[:, 0:nw], lhsT=wgu[:, kk, col0:col0 + 128], rhs=xT_ap(kk, n0, nw), start=(kk == 0), stop=(kk == KCH - 1)),
                         reads=[b_wgu, b_x], writes=[bb_], signal=(kk == KCH - 1))
        for pi, (n0, nw) in enumerate(pieces):
            i = ti % len(tmp); ti += 1
            P.op("scalar", "activation", A(out=tmp[i][:, 0:nw], in_=gb[pi][0][:, 0:nw], func=AF.Silu), reads=[gb[pi][1]], writes=[b_tmp[i]])
            P.op("vector", "tensor_tensor", A(out=aT[:, c, n0:n0 + nw], in0=ub[pi][0][:, 0:nw], in1=tmp[i][:, 0:nw], op=ALU.mult), reads=[ub[pi][1], b_tmp[i]], writes=[b_aT])
        for (_, bb_) in gb + ub: banks.release(bb_)


def phase_H(k):
    nc, P, banks = k.nc, k.P, k.banks
    CAP = k.CAP; NS = CAP // 128
    npieces = (CAP + 511) // 512; NP = CAP // npieces
    with ExitStack() as st:
        def sb(name, shape, dt):
            return st.enter_context(nc.sbuf_tensor(name, list(shape), dt))
        wgu = [sb(f"h_wgu{i}", [128, KC, 1024], BF16) for i in range(2)]; b_wgu = [Buf(), Buf()]
        wd = [sb(f"h_wd{i}", [128, 4, D], BF16) for i in range(2)]; b_wd = [Buf(), Buf()]
        NSTG = 3
        stg = [sb(f"h_stg{i}", [128, 2048], F32) for i in range(NSTG)]; b_stg = [Buf() for _ in range(NSTG)]
        NXE = NS
        xe = [sb(f"h_xe{i}", [128, D], BF16) for i in range(NXE)]; b_xe = [Buf() for _ in range(NXE)]
        xeT = sb("h_xeT", [128, KC, CAP], BF16); b_xeT = Buf()
        aT = sb("h_aT", [128, 4, CAP], BF16); b_aT = Buf()
        tmp = [sb(f"h_tmp{i}", [128, NP], F32) for i in range(3)]; b_tmp = [Buf() for _ in range(3)]
        yrow = [sb(f"h_y{i}", [128, D], BF16) for i in range(2)]; b_y = [Buf(), Buf()]
        cast_engs = ["vector", "scalar", "vector", "scalar"]
        si = [0]
        def prefetch(e):
            w = e % 2
            for pc in range(12):
                i = si[0] % NSTG; ce = cast_engs[si[0] % 4]; si[0] += 1
                if pc < 8:
                    src = k.w_gu[e, pc * 256:(pc + 1) * 256, :].rearrange("(c p) n -> p c n", p=128)
                    dst = wgu[w][:, 2 * pc:2 * pc + 2, :]; sv = stg[i][:].rearrange("p (c n) -> p c n", n=1024); bw = b_wgu[w]
                else:
                    c = pc - 8
                    src = k.w_dn[e, c * 128:(c + 1) * 128, :]
                    dst = wd[w][:, c, :]; sv = stg[i][:]; bw = b_wd[w]
                P.dma("sync", "dma_start", A(out=sv, in_=src), writes=[b_stg[i]])
                if ce == "scalar":
                    P.op("scalar", "activation", A(out=dst, in_=sv, func=AF.Copy), reads=[b_stg[i]], writes=[bw])
                else:
                    P.op(ce, "tensor_copy", A(out=dst, in_=sv), reads=[b_stg[i]], writes=[bw])
                yield
        def run_some(gen, n):
            for _ in range(n):
                try: next(gen)
                except StopIteration: return
        g0 = prefetch(0); run_some(g0, 12)
        yi = 0
        def load_xe(e, s_):
            r0 = e * CAP + s_ * 128
            P.dma("gpsimd", "dma_start", A(out=xe[s_][:], in_=k.xg_d[r0:r0 + 128, :]), reads=[k.b_xg], writes=[b_xe[s_]])
        for s_ in range(NS): load_xe(0, s_)
        for e in range(NE):
            w = e % 2
            gen = prefetch(e + 1) if e + 1 < NE else iter(())
            for s_ in range(NS):
                i = s_
                for half in range(2):
                    bk, bb = banks.get()
                    bkb = bk[:, :].bitcast(BF16)
                    for j in range(8):
                        c = half * 8 + j
                        P.op("tensor", "transpose", A(out=bkb[:, j * 128:(j + 1) * 128], in_=xe[i][:, c * 128:(c + 1) * 128], identity=k.identb[:]),
                             reads=[b_xe[i], k.b_id], writes=[bb], signal=(j == 7))
                    P.op("vector", "tensor_copy", A(out=xeT[:, half * 8:(half + 1) * 8, s_ * 128:(s_ + 1) * 128], in_=bkb.rearrange("p (c t) -> p c t", t=128)), reads=[bb], writes=[b_xeT])
                run_some(gen, 1)
            if e + 1 < NE:
                for s_ in range(NS): load_xe(e + 1, s_)
            ffn_T(P, banks, wgu[w], b_wgu[w], lambda kk, n0, nw: xeT[:, kk, n0:n0 + nw], b_xeT, [(pc * NP, NP) for pc in range(npieces)], aT, b_aT, tmp, b_tmp, KC)
            run_some(gen, 4)
            for s_ in range(NS):
                y = yi % 2; yi += 1
                for n in range(4):
                    bk, bb = banks.get()
                    for c in range(4):
                        P.op("tensor", "matmul", A(bk[:, :], lhsT=aT[:, c, s_ * 128:(s_ + 1) * 128], rhs=wd[w][:, c, n * 512:(n + 1) * 512], start=(c == 0), stop=(c == 3)),
                             reads=[b_aT, b_wd[w]], writes=[bb], signal=(c == 3))
                    eng, nm, a = (("scalar", "activation", A(out=yrow[y][:, n * 512:(n + 1) * 512], in_=bk[:, :], func=AF.Copy)) if n % 2 == 0 else
                                  ("vector", "tensor_copy", A(out=yrow[y][:, n * 512:(n + 1) * 512], in_=bk[:, :])))
                    P.op(eng, nm, a, reads=[bb], writes=[b_y[y]])
                r0 = e * CAP + s_ * 128
                P.dma("gpsimd", "dma_start", A(out=k.yg_d[r0:r0 + 128, :], in_=yrow[y][:]), reads=[b_y[y]], joins=[k.b_yg])
                run_some(gen, 2)
            run_some(gen, 12)
        P.flush()


def phase_I(k):
    nc, P, banks = k.nc, k.P, k.banks
    with ExitStack() as st:
        def sb(name, shape, dt):
            return st.enter_context(nc.sbuf_tensor(name, list(shape), dt))
        g_t = sb("i_g", [128, D], F32); b_t = sb("i_b", [128, D], F32); b_gb = Buf()
        for dst, src in ((g_t, k.ln2_g), (b_t, k.ln2_b)):
            P.dma("sync", "dma_start", A(out=dst[:], in_=bass.AP(src.tensor, src.offset, [[0, 128], [1, D]])), joins=[b_gb])
        hrow = [sb(f"i_h{i}", [128, D], F32) for i in range(2)]; ysh = [sb(f"i_ys{i}", [128, D], F32) for i in range(2)]; b_in = [Buf(), Buf()]
        NY = 12
        yj = [sb(f"i_yj{i}", [128, D], BF16) for i in range(NY)]; b_yj = [Buf() for _ in range(NY)]
        dg = [sb(f"i_dg{i}", [128, 128], BF16) for i in range(NY)]; b_dg = [Buf() for _ in range(NY)]
        acc = [sb(f"i_acc{i}", [128, D], F32) for i in range(2)]; b_acc = [Buf(), Buf()]
        orow = [sb(f"i_o{i}", [128, D], F32) for i in range(2)]; b_o = [Buf(), Buf()]
        stats = sb("i_stats", [128, 4, 6], F32); mv = sb("i_mv", [128, 4], F32); b_st = Buf()
        ji = 0
        for t in range(k.NT):
            i = t % 2; r0 = t * 128
            P.dma("sync", "dma_start", A(out=hrow[i][:], in_=k.h_d[r0:r0 + 128, :]), writes=[b_in[i]])
            P.dma("sync", "dma_start", A(out=ysh[i][:], in_=k.ysh_d[r0:r0 + 128, :]), joins=[b_in[i]])
            P.op("vector", "scalar_tensor_tensor", A(out=acc[i][:], in0=hrow[i][:], scalar=ALPHA, in1=ysh[i][:], op0=ALU.mult, op1=ALU.add), reads=[b_in[i]], writes=[b_acc[i]])
            bks = [banks.get(hold=True) for _ in range(4)]
            for j in range(8):
                q = ji % NY; ji += 1
                P.dma("gpsimd", "indirect_dma_start", A(out=yj[q][:], out_offset=None, in_=k.yg_d, in_offset=bass.IndirectOffsetOnAxis(ap=k.idx[:, t, j:j + 1], axis=0)),
                      reads=[k.b_yg, k.b_idx], writes=[b_yj[q]])
                P.op("vector", "tensor_scalar", A(out=dg[q][:], in0=k.identb[:], scalar1=k.wj[:, t, j:j + 1], scalar2=None, op0=ALU.mult), reads=[k.b_id, k.b_wj], writes=[b_dg[q]])
                for n in range(4):
                    P.op("tensor", "matmul", A(bks[n][0][:, :], lhsT=dg[q][:], rhs=yj[q][:, n * 512:(n + 1) * 512], start=(j == 0), stop=(j == 7)),
                         reads=[b_dg[q], b_yj[q]], writes=[bks[n][1]], signal=(j == 7 or n == 3))
            for n in range(4):
                P.op("vector", "tensor_tensor", A(out=acc[i][:, n * 512:(n + 1) * 512], in0=acc[i][:, n * 512:(n + 1) * 512], in1=bks[n][0][:, :], op=ALU.add),
                     reads=[bks[n][1]], writes=[b_acc[i]])
                banks.release(bks[n][1])
            layer_norm_rows(P, k, acc[i], b_acc[i], orow[i], b_o[i], g_t, b_t, b_gb, stats, mv, b_st, eng2="vector")
            P.dma("sync", "dma_start", A(out=k.out[r0:r0 + 128, :], in_=orow[i][:]), reads=[b_o[i]])
        P.flush()


class K:
    pass


def build_program(NSEQ, S, CAP, debug=(), upto=99):
    T = NSEQ * S
    NG = T // 512
    NT = T // 128
    nc = bass.Bass("TRN2", target_bir_lowering=False)
    k = K(); k.upto = upto; k.debug = debug; k.nc = nc; k.T = T; k.S = S; k.NSEQ = NSEQ; k.NG = NG; k.NT = NT; k.CAP = CAP
    def din(name, shape, dt=F32):
        return nc.dram_tensor(name, list(shape), dt, kind="ExternalInput").ap()
    def dscr(name, shape, dt):
        kind = "ExternalOutput" if name in debug else "Internal"
        return nc.dram_tensor(name, list(shape), dt, kind=kind).ap()
    k.x = din("x", [T, D]); k.pos = din("positions", [T], I32)
    k.w_in = din("w_in", [D, IN_COLS]); k.b_gate = din("b_gate", [2 * D])
    k.q_norm_g = din("q_norm_g", [512]); k.kv_norm_g = din("kv_norm_g", [256])
    k.w_uq = din("w_uq", [512, 1536]); k.w_uk = din("w_uk", [256, 1024]); k.w_uv = din("w_uv", [256, 1024])
    k.sinks = din("swa_sinks", [16]); k.rel_table = din("rel_table", [32, 16])
    k.w_br_mla = din("w_br_mla", [1024, D]); k.w_br_swa = din("w_br_swa", [1024, D]); k.w_out = din("w_out", [D, D])
    k.ln1_g = din("ln1_g", [D]); k.ln1_b = din("ln1_b", [D])
    k.w_router = din("w_router", [D, NE]); k.router_bias = din("router_bias", [NE])
    k.w_gu = din("w_gate_up", [NE, D, 2 * FF]); k.w_dn = din("w_down", [NE, FF, D])
    k.w_sgu = din("w_shared_gate_up", [D, 2 * FF]); k.w_sd = din("w_shared_down", [FF, D])
    k.ln2_g = din("ln2_g", [D]); k.ln2_b = din("ln2_b", [D])
    k.consts = din("consts", [128, 512])
    k.out = nc.dram_tensor("out", [T, D], F32, kind="ExternalOutput").ap()
    k.wb_in = dscr("wb_in", [D, IN_COLS], BF16)
    k.wb_krrot = dscr("wb_krrot", [D, 64], BF16)
    k.wb_uq = dscr("wb_uq", [512, 1536], BF16); k.wb_uqrot = dscr("wb_uqrot", [512, 512], BF16)
    k.wb_uk = dscr("wb_uk", [256, 1024], BF16); k.wb_uv = dscr("wb_uv", [256, 1024], BF16)
    k.wb_brm = dscr("wb_brm", [1024, D], BF16); k.wb_brs = dscr("wb_brs", [1024, D], BF16)
    k.wb_out = dscr("wb_out", [D, D], BF16); k.wb_router = dscr("wb_router", [D, NE], BF16)
    k.wg_d = dscr("wg_d", [32, 128, KC, 128], BF16); k.wbm_d = dscr("wbm_d", [KC, 128, 8, 128], BF16); k.wbs_d = dscr("wbs_d", [KC, 128, 8, 128], BF16)
    k.wb_sgu = dscr("wb_sgu", [D, 2 * FF], BF16); k.wb_sd = dscr("wb_sd", [FF, D], BF16)
    k.xT_d = dscr("xT_d", [D, T], BF16)
    k.qn_d = dscr("qn_d", [1024, T], BF16); k.qr_d = dscr("qr_d", [512, T], BF16)
    k.kn_d = dscr("kn_d", [1024, T], BF16); k.kr_d = dscr("kr_d", [64, T], BF16)
    k.vm_d = dscr("vm_d", [T, 1024], BF16)
    k.qs_d = dscr("qs_d", [1024, T], BF16); k.ks_d = dscr("ks_d", [256, T], BF16); k.vs_d = dscr("vs_d", [T, 256], BF16)
    k.omT_d = dscr("omT_d", [1024, T], BF16); k.osT_d = dscr("osT_d", [1024, T], BF16)
    k.mT_d = dscr("mT_d", [D, T], BF16)
    k.h_d = dscr("h_d", [T, D], F32); k.hb_d = dscr("hb_d", [T, D], BF16); k.hT_d = dscr("hT_d", [D, T], BF16)
    k.tpad_d = dscr("tpad_d", [16, 512], F32)
    k.zf_d = dscr("zf_d", [16 * 128 * 513], F32)
    k.ysh_d = dscr("ysh_d", [T, D], F32)
    k.xg_d = dscr("xg_d", [NE * CAP + 128, D], BF16)
    k.yg_d = dscr("yg_d", [NE * CAP + 128, D], BF16)
    k.dbg_route = dscr("dbg_route", [128, NT * 80], F32)

    with ExitStack() as st:
        P = Prog(nc, st); k.P = P
        k.banks = PsumBanks(nc, st)
        def sb(name, shape, dt):
            return st.enter_context(nc.sbuf_tensor(name, list(shape), dt))
        k.cst = sb("cst", [128, 512], F32); k.b_cst = Buf()
        k.identb = sb("identb", [128, 128], BF16); k.trib = sb("trib", [128, 128], BF16)
        k.onesb = sb("onesb", [128, 128], BF16); k.b_id = Buf()
        k.idx = sb("idx", [128, NT, 8], I32); k.b_idx = Buf()
        k.wj = sb("wj", [128, NT, 8], F32); k.b_wj = Buf()
        k.b_xg = Buf(); k.b_yg = Buf()
        k.epsr = sb("epsr", [128, 1], F32); k.epsl = sb("epsl", [128, 1], F32)
        P.op("vector", "memset", A(k.epsr[:], RMS_EPS), writes=[k.b_id])
        P.op("vector", "memset", A(k.epsl[:], LN_EPS), writes=[k.b_id])
        P.dma("sync", "dma_start", A(out=k.cst[:], in_=k.consts), writes=[k.b_cst])
        P.op("vector", "tensor_copy", A(out=k.identb[:], in_=k.cst[:, 0:128]), reads=[k.b_cst], writes=[k.b_id])
        P.op("vector", "tensor_copy", A(out=k.trib[:], in_=k.cst[:, 128:256]), reads=[k.b_cst], writes=[k.b_id])
        P.op("vector", "memset", A(k.onesb[:], 1.0), writes=[k.b_id])
        k.zrowb = sb("zrowb", [128, D], BF16); k.b_z = Buf()
        P.op("gpsimd", "memset", A(k.zrowb[:], 0.0), writes=[k.b_z])
        phase_W(k, 1)
        phase_A(k)
        if k.upto >= 2: phase_B(k)
        if k.upto >= 3: phase_C(k)
        if k.upto >= 4: phase_D(k)
        with ExitStack() as st_r:
            k.Wc = st_r.enter_context(nc.sbuf_tensor("Wc", [128, NT, NE], F32)); k.b_Wc = Buf()
            k.Msk = st_r.enter_context(nc.sbuf_tensor("Msk", [128, NT, NE], F32)); k.b_Msk = Buf()
            if k.upto >= 5: phase_E(k)
            if k.upto >= 6: phase_FG(k)
        if k.upto >= 8: phase_H(k)
        if k.upto >= 9: phase_I(k)
        P.flush(final=True)
    return nc


def make_in_map(inputs, b0, nseq, S, consts):
    m = {}
    m["x"] = np.ascontiguousarray(inputs["x"][b0:b0 + nseq, :S].reshape(nseq * S, D))
    m["positions"] = np.ascontiguousarray(inputs["positions"][b0:b0 + nseq, :S].reshape(nseq * S)).astype(np.int32)
    for name in ("w_in", "q_norm_g", "kv_norm_g", "w_uq", "w_uk", "w_uv", "swa_sinks", "w_br_mla", "w_br_swa", "w_out",
                 "ln1_g", "ln1_b", "w_router", "router_bias", "w_gate_up", "w_down", "w_shared_gate_up", "w_shared_down",
                 "ln2_g", "ln2_b"):
        m[name] = np.ascontiguousarray(inputs[name][0])
    m["b_gate"] = np.ascontiguousarray(inputs["b_gate"][0].reshape(-1))
    m["rel_table"] = np.ascontiguousarray(inputs["rel_table"])
    m["consts"] = consts
    return m


def kernel(**inputs):
    NCORES = 8; NSEQ = 2; S = 2048; CAP = 768
    inputs = {k_: np.asarray(v) for k_, v in inputs.items()}
    nc = build_program(NSEQ, S, CAP)
    consts = make_consts()
    in_maps = [make_in_map(inputs, c * NSEQ, NSEQ, S, consts) for c in range(NCORES)]
    res = run_bass_kernel_spmd(nc, in_maps, core_ids=list(range(NCORES)))
    outs = [np.asarray(r["out"]).reshape(NSEQ, S, D) for r in res.results]
    return np.concatenate(outs, axis=0).astype(np.float32)
```

```python
import math
import numpy as np
from contextlib import ExitStack
import concourse.bass as bass
import concourse.mybir as mybir
from concourse.bass_utils import run_bass_kernel_spmd

F32 = mybir.dt.float32; BF16 = mybir.dt.bfloat16; I32 = mybir.dt.int32; U32 = mybir.dt.uint32
AF = mybir.ActivationFunctionType; ALU = mybir.AluOpType; AX = mybir.AxisListType

D = 2048; KC = 16
OFF_CQ, OFF_CKV, OFF_KR, OFF_QS, OFF_KS, OFF_VS, OFF_GATE = 0, 512, 768, 832, 1856, 2112, 2368
NA = 2368
IN_COLS = 6464
NE = 64; FF = 512; TOPK = 8
ALPHA = 2.0 ** 0.25
LN_EPS = 1e-5; RMS_EPS = 1e-6
NEG = -30000.0
TWO_PI = 2.0 * math.pi


def A(*a, **kw):
    return (a, kw)


def _bind(fn, args):
    if isinstance(fn, str):
        a, kw = args
        return lambda e: getattr(e, fn)(*a, **kw)
    return fn


class Buf:
    __slots__ = ("name", "writers", "readers", "gen_deps")
    def __init__(self, name=""):
        self.name = name; self.writers = []; self.readers = []; self.gen_deps = set()


class Prog:
    ENGS = ("tensor", "vector", "scalar", "gpsimd", "sync")
    NDMA = 14
    def __init__(self, nc, stack):
        self.nc = nc
        self.ops = {e: [] for e in self.ENGS}
        self.sem = {e: stack.enter_context(nc.semaphore("c_" + e)) for e in self.ENGS}
        self.cnt = {e: 0 for e in self.ENGS}
        self.dsem = {}; self.duse = {}; self.dnext = {}
        for q in ("sync", "gpsimd", "scalar"):
            self.dsem[q] = [stack.enter_context(nc.semaphore(f"d_{q}{i}")) for i in range(self.NDMA)]
            self.duse[q] = [0] * self.NDMA
            self.dnext[q] = 0
        self.waited = {e: {} for e in self.ENGS}
        self.pending = {e: set() for e in self.ENGS}
        self.nops = 0
    def _deps(self, eng, reads, writes, extra, joins=()):
        deps = set(t for t in extra if t is not None)
        for b in reads:
            deps.update(b.writers)
        for b in writes:
            deps.update(b.writers); deps.update(b.readers)
        for b in joins:
            deps.update(b.gen_deps); deps.update(b.readers)
        if self.pending[eng]:
            deps |= self.pending[eng]; self.pending[eng] = set()
        deps = {t for t in deps if not (t[0] == "c" and t[1] == eng and t[2] > self.cnt[eng])}
        return deps
    def _commit(self, tok, reads, writes, joins=()):
        for b in reads: b.readers.append(tok)
        for b in writes:
            b.gen_deps = set(b.writers) | set(b.readers)
            b.writers = [tok]; b.readers = []
        for b in joins:
            b.writers.append(tok)
    def op(self, eng, fn, args=None, reads=(), writes=(), signal=True, deps=(), joins=()):
        fn = _bind(fn, args)
        d = self._deps(eng, reads, writes, deps, joins)
        if signal:
            self.cnt[eng] += 1
            tok = ("c", eng, self.cnt[eng])
        else:
            tok = ("c", eng, self.cnt[eng] + 1)
        self.ops[eng].append(("op", fn, d, signal))
        self._commit(tok, reads, writes, joins)
        self.nops += 1
        return tok
    def dma(self, q, fn, args=None, reads=(), writes=(), deps=(), joins=()):
        fn = _bind(fn, args)
        d = self._deps(q, reads, writes, deps, joins)
        i = self.dnext[q]; self.dnext[q] = (i + 1) % self.NDMA
        prev = self.duse[q][i]
        if prev > 0: d.add(("d", q, i, prev * 16))
        self.duse[q][i] = prev + 1
        tok = ("d", q, i, (prev + 1) * 16)
        self.ops[q].append(("dma", fn, d, (q, i)))
        self._commit(tok, reads, writes, joins)
        self.nops += 1
        return tok
    def all_tokens(self, final=True):
        toks = set()
        for e in self.ENGS:
            if self.cnt[e] > 0: toks.add(("c", e, self.cnt[e]))
        for q in self.dsem:
            if q == "scalar" and not final: continue
            for i in range(self.NDMA):
                if self.duse[q][i] > 0: toks.add(("d", q, i, self.duse[q][i] * 16))
        return toks
    def flush(self, final=False):
        toks = self.all_tokens(final)
        with self.nc.Block() as block:
            def build(engname):
                def body(eng):
                    waited = self.waited[engname]
                    def do_wait(tok):
                        if tok[0] == "c":
                            key = ("c", tok[1]); val = tok[2]; sem = self.sem[tok[1]]
                        else:
                            key = ("d", tok[1], tok[2]); val = tok[3]; sem = self.dsem[tok[1]][tok[2]]
                        if waited.get(key, 0) >= val: return
                        waited[key] = val
                        eng.wait_ge(sem, val)
                    for kind, fn, deps, info in self.ops[engname]:
                        for tok in sorted(deps, key=str): do_wait(tok)
                        ins = fn(eng)
                        if kind == "op":
                            if info: ins.then_inc(self.sem[engname], 1)
                        else:
                            q, i = info
                            ins.then_inc(self.dsem[q][i], 16)
                    if final and engname == "sync":
                        for tok in sorted(toks, key=str): do_wait(tok)
                return body
            block.tensor(build("tensor")); block.vector(build("vector")); block.scalar(build("scalar"))
            block.gpsimd(build("gpsimd")); block.sync(build("sync"))
        self.ops = {e: [] for e in self.ENGS}
        for e in self.ENGS: self.pending[e] = set(toks)


class PsumBanks:
    def __init__(self, nc, stack, n=8):
        self.t = [stack.enter_context(nc.psum_tensor(f"bank{i}", [128, 512], F32)) for i in range(n)]
        self.b = [Buf(f"bank{i}") for i in range(n)]
        self.i = 0; self.n = n; self.held = set()
    def get(self, hold=False):
        while self.i in self.held:
            self.i = (self.i + 1) % self.n
        i = self.i; self.i = (i + 1) % self.n
        if hold: self.held.add(i)
        return self.t[i], self.b[i]
    def release(self, buf):
        self.held.discard(self.b.index(buf))


def t5_bucket_np(n):
    n = np.maximum(n, 0)
    max_exact = 16
    large = max_exact + (np.log(np.maximum(n, 1).astype(np.float32) / np.float32(max_exact))
                         / np.float32(math.log(128 / max_exact)) * np.float32(32 - max_exact)).astype(np.int32)
    large = np.minimum(large, 31)
    return np.where(n < max_exact, n, large)


def make_consts():
    c = np.zeros((128, 512), np.float32)
    c[:, 0:128] = np.eye(128, dtype=np.float32)
    k = np.arange(128)[:, None]; q = np.arange(128)[None, :]
    c[:, 128:256] = (k <= q).astype(np.float32)
    inv = (10000.0 ** (-np.arange(0, 64, 2, dtype=np.float32) / 64)).astype(np.float32)
    c[:, 256] = np.tile(inv, 4)
    oh = np.zeros((32, 128), np.float32)
    oh[t5_bucket_np(np.arange(128)), np.arange(128)] = 1.0
    c[0:32, 257:385] = oh
    c[:, 385] = np.arange(128, dtype=np.float32)
    return c


def phase_W(k, part, st_ext=None):
    nc, P = k.nc, k.P
    with ExitStack() as st_own:
        st = st_own if st_ext is None else st_ext
        dq = "sync" if part == 1 else "gpsimd"
        CB = 3232
        stg = [st.enter_context(nc.sbuf_tensor(f"w{part}_stg{i}", [128, CB], F32)) for i in range(3)]
        outb = [st.enter_context(nc.sbuf_tensor(f"w{part}_out{i}", [128, CB], BF16)) for i in range(3)]
        rot = [st.enter_context(nc.sbuf_tensor(f"w{part}_rot{i}", [128, 512], BF16)) for i in range(2)]
        b_stg = [Buf() for _ in range(3)]; b_out = [Buf() for _ in range(3)]; b_rot = [Buf() for _ in range(2)]
        engs = ["vector", "gpsimd", "scalar"] if part == 1 else ["gpsimd", "gpsimd", "gpsimd"]
        step = [0]; rstep = [0]; pend = []
        def cast(eng, out, in_):
            if eng == "scalar":
                return "activation", A(out=out, in_=in_, func=AF.Copy)
            return "tensor_copy", A(out=out, in_=in_)
        def do(src, dst, R, C, special=None, dstfn=None):
            nrc = (R + 127) // 128
            for rc in range(nrc):
                r0 = rc * 128; pr = min(128, R - r0)
                for c0 in range(0, C, CB):
                    cw = min(CB, C - c0)
                    i = step[0] % 3; step[0] += 1
                    P.dma(dq, "dma_start", A(out=stg[i][0:pr, 0:cw], in_=src[r0:r0 + pr, c0:c0 + cw]),
                          writes=[b_stg[i]])
                    def finish(i=i, r0=r0, pr=pr, c0=c0, cw=cw, rc=rc):
                        P.op(engs[i], *cast(engs[i], outb[i][0:pr, 0:cw], stg[i][0:pr, 0:cw]), reads=[b_stg[i]], writes=[b_out[i]])
                        if dstfn is None:
                            P.dma(dq, "dma_start", A(out=dst[r0:r0 + pr, c0:c0 + cw], in_=outb[i][0:pr, 0:cw]), reads=[b_out[i]])
                        else:
                            P.dma(dq, "dma_start", A(out=dstfn(rc, c0, cw), in_=outb[i][0:pr, 0:cw].rearrange("p (c n) -> p c n", n=128)), reads=[b_out[i]])
                        if special is not None and c0 == 0:
                            special(i, r0)
                    if pend: pend.pop()()
                    pend.append(finish)
        def sp_in(i, r0):
            j = rstep[0] % 2; rstep[0] += 1
            P.op("vector", "tensor_scalar", A(out=rot[j][:, 0:32], in0=stg[i][:, OFF_KR + 32:OFF_KR + 64], scalar1=-1.0, scalar2=None, op0=ALU.mult),
                 reads=[b_stg[i]], writes=[b_rot[j]])
            P.op("vector", "tensor_copy", A(out=rot[j][:, 32:64], in_=stg[i][:, OFF_KR:OFF_KR + 32]), reads=[b_stg[i]], writes=[b_rot[j]])
            P.dma(dq, "dma_start", A(out=k.wb_krrot[r0:r0 + 128, :], in_=rot[j][:, 0:64]), reads=[b_rot[j]])
        def sp_uq(i, r0):
            j = rstep[0] % 2; rstep[0] += 1
            sv = stg[i][:, 0:1536].rearrange("p (h d) -> p h d", d=192)
            rv = rot[j][:, 0:512].rearrange("p (h d) -> p h d", d=64)
            P.op("vector", "tensor_scalar", A(out=rv[:, :, 0:32], in0=sv[:, :, 160:192], scalar1=-1.0, scalar2=None, op0=ALU.mult),
                 reads=[b_stg[i]], writes=[b_rot[j]])
            P.op("vector", "tensor_copy", A(out=rv[:, :, 32:64], in_=sv[:, :, 128:160]), reads=[b_stg[i]], writes=[b_rot[j]])
            P.dma(dq, "dma_start", A(out=k.wb_uqrot[r0:r0 + 128, :], in_=rot[j][:, 0:512]), reads=[b_rot[j]])
        if part == 1:
            do(k.w_in[:, 0:NA], k.wb_in[:, 0:NA], D, NA, sp_in)
            do(k.w_uq, k.wb_uq, 512, 1536, sp_uq)
            do(k.w_uk, k.wb_uk, 256, 1024); do(k.w_uv, k.wb_uv, 256, 1024)
            if pend: pend.pop()()
            P.flush()
            return
        for half in range(2):
            do(k.w_in[:, OFF_GATE + half * D:OFF_GATE + (half + 1) * D], None, D, D,
               dstfn=lambda rc, c0, cw, half=half: k.wg_d[half * KC + c0 // 128: half * KC + (c0 + cw) // 128, :, rc, :].rearrange("c p n -> p c n"))
        do(k.w_br_mla, None, 1024, D, dstfn=lambda rc, c0, cw: k.wbm_d[c0 // 128:(c0 + cw) // 128, :, rc, :].rearrange("c p n -> p c n"))
        do(k.w_br_swa, None, 1024, D, dstfn=lambda rc, c0, cw: k.wbs_d[c0 // 128:(c0 + cw) // 128, :, rc, :].rearrange("c p n -> p c n"))
        do(k.w_out, k.wb_out, D, D); do(k.w_router, k.wb_router, D, NE)
        do(k.w_sgu, k.wb_sgu, D, 2 * FF); do(k.w_sd, k.wb_sd, FF, D)
        if pend: pend.pop()()


def rope_tables(k, st, P, t0, cosT, sinT, b_cs, tmp):
    nc = k.nc
    posi, posf, ang, kf, ki, r = tmp
    b = Buf()
    pos_bc = bass.AP(k.pos.tensor, k.pos.offset + t0, [[0, 64], [1, 512]])
    P.dma("sync", "dma_start", A(out=posi[:], in_=pos_bc), writes=[b])
    P.op("vector", "tensor_copy", A(out=posf[:], in_=posi[:]), reads=[b], writes=[b])
    P.op("vector", "tensor_scalar", A(out=ang[:], in0=posf[:], scalar1=k.cst[0:64, 256:257], scalar2=None, op0=ALU.mult),
         reads=[b, k.b_cst], writes=[b])
    for which, dst in ((0, sinT), (1, cosT)):
        src = ang
        if which == 1:
            P.op("vector", "tensor_scalar", A(out=posf[:], in0=ang[:], scalar1=math.pi / 2, scalar2=None, op0=ALU.add), reads=[b], writes=[b])
            src = posf
        P.op("vector", "tensor_scalar", A(out=kf[:], in0=src[:], scalar1=1.0 / TWO_PI, scalar2=None, op0=ALU.mult), reads=[b], writes=[b])
        P.op("vector", "tensor_copy", A(out=ki[:], in_=kf[:]), reads=[b], writes=[b])
        P.op("vector", "tensor_copy", A(out=kf[:], in_=ki[:]), reads=[b], writes=[b])
        P.op("vector", "scalar_tensor_tensor", A(out=r[:], in0=kf[:], scalar=-TWO_PI, in1=src[:], op0=ALU.mult, op1=ALU.add), reads=[b], writes=[b])
        P.op("vector", "tensor_scalar", A(out=kf[:], in0=r[:], scalar1=math.pi, scalar2=-TWO_PI, op0=ALU.is_gt, op1=ALU.mult), reads=[b], writes=[b])
        P.op("vector", "tensor_tensor", A(out=r[:], in0=r[:], in1=kf[:], op=ALU.add), reads=[b], writes=[b])
        P.op("vector", "tensor_scalar", A(out=r[:], in0=r[:], scalar1=-math.pi, scalar2=math.pi, op0=ALU.max, op1=ALU.min), reads=[b], writes=[b])
        P.op("scalar", "activation", A(out=dst[:], in_=r[:], func=AF.Sin), reads=[b], writes=[b_cs])
        b.writers = list(b_cs.writers)


def phase_A(k):
    nc, P, banks = k.nc, k.P, k.banks
    T = k.T
    with ExitStack() as st:
        def sb(name, shape, dt):
            return st.enter_context(nc.sbuf_tensor(name, list(shape), dt))
        wA = sb("wA", [128, KC, NA], BF16); wkrr = sb("wkrr", [128, KC, 64], BF16)
        wuq = sb("wuq", [128, 4, 1536], BF16); wuqr = sb("wuqr", [128, 4, 512], BF16)
        wuk = sb("wuk", [128, 2, 1024], BF16); wuv = sb("wuv", [128, 2, 1024], BF16)
        gq = sb("gq", [128, 4], F32); gkv = sb("gkv", [128, 2], F32)
        b_w = Buf()
        P.dma("sync", "dma_start", A(out=wA[:], in_=k.wb_in[:, 0:NA].rearrange("(c p) n -> p c n", p=128)), writes=[b_w])
        P.dma("sync", "dma_start", A(out=wkrr[:], in_=k.wb_krrot.rearrange("(c p) n -> p c n", p=128)), writes=[b_w])
        P.dma("sync", "dma_start", A(out=wuq[:], in_=k.wb_uq.rearrange("(c p) n -> p c n", p=128)), writes=[b_w])
        P.dma("sync", "dma_start", A(out=wuqr[:], in_=k.wb_uqrot.rearrange("(c p) n -> p c n", p=128)), writes=[b_w])
        P.dma("sync", "dma_start", A(out=wuk[:], in_=k.wb_uk.rearrange("(c p) n -> p c n", p=128)), writes=[b_w])
        P.dma("sync", "dma_start", A(out=wuv[:], in_=k.wb_uv.rearrange("(c p) n -> p c n", p=128)), writes=[b_w])
        P.dma("sync", "dma_start", A(out=gq[:], in_=k.q_norm_g.rearrange("(c p) -> p c", p=128), allow_slow_non_contiguous=True), writes=[b_w])
        P.dma("sync", "dma_start", A(out=gkv[:], in_=k.kv_norm_g.rearrange("(c p) -> p c", p=128), allow_slow_non_contiguous=True), writes=[b_w])
        xrow = [sb("xrow0", [128, D], F32)] * 2; b_xrow = [Buf()] * 2
        xb = [sb(f"xb{i}", [128, D], BF16) for i in range(2)]; b_xb = [Buf(), Buf()]
        xT = sb("xT", [128, KC, 512], BF16); b_xT = Buf()
        cqn = sb("cqn", [128, 4, 512], BF16); b_cqn = Buf()
        ckvn = sb("ckvn", [128, 2, 512], BF16); b_ckvn = Buf()
        sq = [sb(f"sq{i}", [128, 512], BF16) for i in range(4)]; b_sq = [Buf() for _ in range(4)]
        rstd = sb("rstd", [128, 512], F32); b_rstd = Buf()
        cosT = sb("cosT", [64, 512], F32); sinT = sb("sinT", [64, 512], F32); b_cs = Buf()
        tmp = (sb("posi", [64, 512], I32), sb("posf", [64, 512], F32), sb("ang", [64, 512], F32),
               sb("kf", [64, 512], F32), sb("ki", [64, 512], I32), sb("rr", [64, 512], F32))
        t1 = sb("t1", [64, 512], F32); t2 = sb("t2", [64, 512], F32); b_t = Buf()
        stgs = [sb(f"stgA{i}", [128, 8, 512], BF16) for i in range(2)]; b_stgs = [Buf(), Buf()]
        stg_i = [0]
        def next_stage():
            i = stg_i[0] % 2; stg_i[0] += 1
            return stgs[i], b_stgs[i]
        stkr = sb("stkr", [64, 512], BF16); b_stkr = Buf()
        evac_i = [0]
        def evac(out, in_, reads, writes):
            evac_i[0] += 1
            if evac_i[0] % 2 == 0:
                return P.op("scalar", "activation", A(out=out, in_=in_, func=AF.Copy), reads=reads, writes=writes)
            return P.op("vector", "tensor_copy", A(out=out, in_=in_), reads=reads, writes=writes)
        def acc(out_ap, pairs, reads, b_out):
            n = len(pairs)
            for i, (l, r) in enumerate(pairs):
                P.op("tensor", "matmul", A(out_ap, lhsT=l, rhs=r, start=(i == 0), stop=(i == n - 1)),
                     reads=reads, writes=[b_out], signal=(i == n - 1))
        for g in range(k.NG):
            t0 = g * 512
            rope_tables(k, st, P, t0, cosT, sinT, b_cs, tmp)
            for tt in range(4):
                s = tt % 2
                r0 = t0 + tt * 128
                P.dma("sync", "dma_start", A(out=xrow[s][:], in_=k.x[r0:r0 + 128, :]), writes=[b_xrow[s]])
                P.op("gpsimd", "tensor_copy", A(out=xb[s][:, 0:1024], in_=xrow[s][:, 0:1024]), reads=[b_xrow[s]], writes=[b_xb[s]])
                P.op("vector", "tensor_copy", A(out=xb[s][:, 1024:2048], in_=xrow[s][:, 1024:2048]), reads=[b_xrow[s]], writes=[b_xb[s]])
                for half in range(2):
                    bk, bb = banks.get()
                    bkb = bk[:, :].bitcast(BF16)
                    for j in range(8):
                        c = half * 8 + j
                        P.op("tensor", "transpose", A(out=bkb[:, j * 128:(j + 1) * 128], in_=xb[s][:, c * 128:(c + 1) * 128], identity=k.identb[:]),
                             reads=[b_xb[s], k.b_id], writes=[bb], signal=(j == 7))
                    evac(xT[:, half * 8:(half + 1) * 8, tt * 128:(tt + 1) * 128], bkb.rearrange("p (c t) -> p c t", t=128), [bb], [b_xT])
            P.dma("sync", "dma_start", A(out=k.xT_d[:, t0:t0 + 512].rearrange("(c p) t -> p c t", p=128), in_=xT[:]), reads=[b_xT])
            for (off, nch, gvec, dst, b_dst, nfeat) in ((OFF_CQ, 4, gq, cqn, b_cqn, 512.0), (OFF_CKV, 2, gkv, ckvn, b_ckvn, 256.0)):
                held = []
                for m in range(nch):
                    bk, bb = banks.get(hold=True)
                    acc(bk[:, :], [(wA[:, kk, off + m * 128: off + (m + 1) * 128], xT[:, kk, :]) for kk in range(KC)], [b_w, b_xT], bb)
                    P.op("scalar", "activation", A(out=sq[m][:], in_=bk[:, :], func=AF.Square), reads=[bb], writes=[b_sq[m]])
                    held.append((bk, bb))
                bs, bbs = banks.get()
                acc(bs[:, :], [(k.onesb[:], sq[m][:]) for m in range(nch)], [k.b_id] + b_sq[:nch], bbs)
                P.op("scalar", "activation", A(out=rstd[:], in_=bs[:, :], func=AF.Sqrt, scale=1.0 / nfeat, bias=k.epsr[:]), reads=[bbs, k.b_id], writes=[b_rstd])
                P.op("vector", "reciprocal", A(out=rstd[:], in_=rstd[:]), reads=[b_rstd], writes=[b_rstd])
                for m in range(nch):
                    bk, bb = held[m]
                    P.op("vector", "scalar_tensor_tensor", A(out=dst[:, m, :], in0=bk[:, :], scalar=gvec[:, m:m + 1], in1=rstd[:], op0=ALU.mult, op1=ALU.mult),
                         reads=[bb, b_rstd, b_w], writes=[b_dst])
                    banks.release(bb)
            stq, b_stq = next_stage(); str_, b_str = next_stage()
            for h in range(8):
                bk, bb = banks.get()
                acc(bk[:, :], [(wuq[:, kk, h * 192:h * 192 + 128], cqn[:, kk, :]) for kk in range(4)], [b_w, b_cqn], bb)
                evac(stq[:, h, :], bk[:, :], [bb], [b_stq])
                bA, bbA = banks.get(); bB, bbB = banks.get()
                acc(bA[0:64, :], [(wuq[:, kk, h * 192 + 128:h * 192 + 192], cqn[:, kk, :]) for kk in range(4)], [b_w, b_cqn], bbA)
                acc(bB[0:64, :], [(wuqr[:, kk, h * 64:(h + 1) * 64], cqn[:, kk, :]) for kk in range(4)], [b_w, b_cqn], bbB)
                P.op("vector", "tensor_tensor", A(out=t1[:], in0=bA[0:64, :], in1=cosT[:], op=ALU.mult), reads=[bbA, b_cs], writes=[b_t])
                P.op("vector", "tensor_tensor", A(out=t2[:], in0=bB[0:64, :], in1=sinT[:], op=ALU.mult), reads=[bbB, b_cs, b_t], writes=[b_t])
                P.op("gpsimd", "tensor_tensor", A(out=str_[0:64, h, :], in0=t1[:], in1=t2[:], op=ALU.add), reads=[b_t], writes=[b_str])
            P.dma("sync", "dma_start", A(out=k.qn_d[:, t0:t0 + 512].rearrange("(h p) t -> p h t", p=128), in_=stq[:]), reads=[b_stq])
            P.dma("sync", "dma_start", A(out=k.qr_d[:, t0:t0 + 512].rearrange("(h p) t -> p h t", p=64), in_=str_[0:64, :, :]), reads=[b_str])
            stk, b_stk = next_stage()
            for h in range(8):
                bk, bb = banks.get()
                acc(bk[:, :], [(wuk[:, kk, h * 128:(h + 1) * 128], ckvn[:, kk, :]) for kk in range(2)], [b_w, b_ckvn], bb)
                evac(stk[:, h, :], bk[:, :], [bb], [b_stk])
            P.dma("sync", "dma_start", A(out=k.kn_d[:, t0:t0 + 512].rearrange("(h p) t -> p h t", p=128), in_=stk[:]), reads=[b_stk])
            stv_, b_stv = next_stage()
            stv = stv_[:].rearrange("p a b -> p (a b)").rearrange("p (t n) -> p t n", n=1024)
            for tt in range(4):
                for half in range(2):
                    bk, bb = banks.get()
                    acc(bk[:, :], [(ckvn[:, kk, tt * 128:(tt + 1) * 128], wuv[:, kk, half * 512:(half + 1) * 512]) for kk in range(2)], [b_w, b_ckvn], bb)
                    evac(stv[:, tt, half * 512:(half + 1) * 512], bk[:, :], [bb], [b_stv])
            P.dma("sync", "dma_start", A(out=k.vm_d[t0:t0 + 512, :].rearrange("(tt p) n -> p tt n", p=128), in_=stv), reads=[b_stv])
            bA, bbA = banks.get(); bB, bbB = banks.get()
            acc(bA[0:64, :], [(wA[:, kk, OFF_KR:OFF_KR + 64], xT[:, kk, :]) for kk in range(KC)], [b_w, b_xT], bbA)
            acc(bB[0:64, :], [(wkrr[:, kk, :], xT[:, kk, :]) for kk in range(KC)], [b_w, b_xT], bbB)
            P.op("vector", "tensor_tensor", A(out=t1[:], in0=bA[0:64, :], in1=cosT[:], op=ALU.mult), reads=[bbA, b_cs], writes=[b_t])
            P.op("vector", "tensor_tensor", A(out=t2[:], in0=bB[0:64, :], in1=sinT[:], op=ALU.mult), reads=[bbB, b_cs, b_t], writes=[b_t])
            P.op("gpsimd", "tensor_tensor", A(out=stkr[:], in0=t1[:], in1=t2[:], op=ALU.add), reads=[b_t], writes=[b_stkr])
            P.dma("sync", "dma_start", A(out=k.kr_d[:, t0:t0 + 512], in_=stkr[:]), reads=[b_stkr])
            stq, b_stq = next_stage()
            for m in range(8):
                bk, bb = banks.get()
                acc(bk[:, :], [(wA[:, kk, OFF_QS + m * 128:OFF_QS + (m + 1) * 128], xT[:, kk, :]) for kk in range(KC)], [b_w, b_xT], bb)
                evac(stq[:, m, :], bk[:, :], [bb], [b_stq])
            P.dma("sync", "dma_start", A(out=k.qs_d[:, t0:t0 + 512].rearrange("(h p) t -> p h t", p=128), in_=stq[:]), reads=[b_stq])
            stk, b_stk = next_stage()
            for m in range(2):
                bk, bb = banks.get()
                acc(bk[:, :], [(wA[:, kk, OFF_KS + m * 128:OFF_KS + (m + 1) * 128], xT[:, kk, :]) for kk in range(KC)], [b_w, b_xT], bb)
                evac(stk[:, m, :], bk[:, :], [bb], [b_stk])
            P.dma("sync", "dma_start", A(out=k.ks_d[:, t0:t0 + 512].rearrange("(h p) t -> p h t", p=128), in_=stk[:, 0:2, :]), reads=[b_stk])
            sts_, b_sts = next_stage()
            sts = sts_[:].rearrange("p a b -> p (a b)")[:, 0:1024].rearrange("p (t n) -> p t n", n=256)
            for tt in range(4):
                bk, bb = banks.get()
                acc(bk[:, 0:256], [(xT[:, kk, tt * 128:(tt + 1) * 128], wA[:, kk, OFF_VS:OFF_VS + 256]) for kk in range(KC)], [b_w, b_xT], bb)
                evac(sts[:, tt, :], bk[:, 0:256], [bb], [b_sts])
            P.dma("sync", "dma_start", A(out=k.vs_d[t0:t0 + 512, :].rearrange("(tt p) n -> p tt n", p=128), in_=sts), reads=[b_sts])
        P.flush()


def phase_B(k):
    nc, P, banks = k.nc, k.P, k.banks
    S = k.S; NKT = S // 128; NQG = S // 512
    scale = 192.0 ** -0.5
    with ExitStack() as st:
        def sb(name, shape, dt):
            return st.enter_context(nc.sbuf_tensor(name, list(shape), dt))
        qn = [sb(f"b_qn{i}", [128, S], BF16) for i in range(2)]; qr = [sb(f"b_qr{i}", [64, S], BF16) for i in range(2)]
        kn = [sb(f"b_kn{i}", [128, S], BF16) for i in range(2)]; vv = [sb(f"b_v{i}", [128, NKT, 128], BF16) for i in range(2)]
        b_in = [Buf(), Buf()]
        kr = sb("b_kr", [64, S], BF16); b_kr = Buf()
        NET = 5
        et = [sb(f"b_et{i}", [128, 512], BF16) for i in range(NET)]; b_et = [Buf() for _ in range(NET)]
        rc = sb("b_rc", [128, 512], F32); b_rc = Buf()
        om = [sb(f"b_om{i}", [128, 512], BF16) for i in range(2)]; b_om = [Buf(), Buf()]
        nrows = NE * k.CAP + 128
        first = True
        for r0 in range(0, nrows, 1024):
            a = min(1024, nrows - r0) // 128
            P.dma("gpsimd", "dma_start", A(out=k.xg_d[r0:r0 + a * 128, :].rearrange("(a p) d -> p a d", p=128), in_=k.zrowb[:].unsqueeze(1).broadcast_to([128, a, D])),
                  reads=[k.b_z], **(dict(writes=[k.b_xg]) if first else dict(joins=[k.b_xg])))
            first = False
        P.dma("gpsimd", "dma_start", A(out=k.yg_d[NE * k.CAP:NE * k.CAP + 128, :], in_=k.zrowb[:]), reads=[k.b_z], writes=[k.b_yg])
        phase_W(k, 2, st)
        it = 0; ei = 0; oi = 0
        for sq_ in range(k.NSEQ):
            tb = sq_ * S
            P.dma("sync", "dma_start", A(out=kr[:], in_=k.kr_d[:, tb:tb + S]), writes=[b_kr])
            for h in range(8):
                i = it % 2; it += 1
                P.dma("sync", "dma_start", A(out=qn[i][:], in_=k.qn_d[h * 128:(h + 1) * 128, tb:tb + S]), writes=[b_in[i]])
                P.dma("sync", "dma_start", A(out=qr[i][:], in_=k.qr_d[h * 64:(h + 1) * 64, tb:tb + S]), joins=[b_in[i]])
                P.dma("sync", "dma_start", A(out=kn[i][:], in_=k.kn_d[h * 128:(h + 1) * 128, tb:tb + S]), joins=[b_in[i]])
                P.dma("sync", "dma_start", A(out=vv[i][:], in_=k.vm_d[tb:tb + S, h * 128:(h + 1) * 128].rearrange("(j p) n -> p j n", p=128)), joins=[b_in[i]])
                for G in range(NQG):
                    q0 = G * 512
                    bo, bbo = banks.get(hold=True); bsum, bbs = banks.get(hold=True)
                    nkt = 4 * G + 4
                    def c0_of(j):
                        r = j - 4 * G
                        return 128 * r if r > 0 else 0
                    staged = {}
                    def stage1(j):
                        nonlocal ei
                        c0 = c0_of(j)
                        bs_, bbs_ = banks.get()
                        P.op("tensor", "matmul", A(bs_[:, c0:512], lhsT=kn[i][:, j * 128:(j + 1) * 128], rhs=qn[i][:, q0 + c0:q0 + 512], start=True, stop=False),
                             reads=[b_in[i]], writes=[bbs_], signal=False)
                        P.op("tensor", "matmul", A(bs_[:, c0:512], lhsT=kr[:, j * 128:(j + 1) * 128], rhs=qr[i][:, q0 + c0:q0 + 512], start=False, stop=True),
                             reads=[b_in[i], b_kr], writes=[bbs_])
                        e = ei % NET; ei += 1
                        P.op("scalar", "activation", A(out=et[e][:, c0:512], in_=bs_[:, c0:512], func=AF.Exp, scale=scale), reads=[bbs_], writes=[b_et[e]])
                        if j - 4 * G >= 0:
                            P.op("vector", "tensor_tensor", A(out=et[e][:, c0:c0 + 128], in0=et[e][:, c0:c0 + 128], in1=k.trib[:], op=ALU.mult),
                                 reads=[k.b_id], writes=[b_et[e]])
                        staged[j] = e
                    def stage2(j):
                        c0 = c0_of(j); e = staged.pop(j)
                        first = (j == 0); last = (j == nkt - 1)
                        P.op("tensor", "matmul", A(bo[:, c0:512], lhsT=vv[i][:, j, :], rhs=et[e][:, c0:512], start=first, stop=last),
                             reads=[b_in[i], b_et[e]], writes=[bbo], signal=False)
                        P.op("tensor", "matmul", A(bsum[:, c0:512], lhsT=k.onesb[:], rhs=et[e][:, c0:512], start=first, stop=last),
                             reads=[k.b_id, b_et[e]], writes=[bbs], signal=True)
                    LOOK = 3
                    for j in range(min(LOOK, nkt)): stage1(j)
                    for j in range(nkt):
                        if j + LOOK < nkt: stage1(j + LOOK)
                        stage2(j)
                    P.op("vector", "reciprocal", A(out=rc[:], in_=bsum[:, :]), reads=[bbs], writes=[b_rc])
                    o = oi % 2; oi += 1
                    P.op("vector", "tensor_tensor", A(out=om[o][:], in0=bo[:, :], in1=rc[:], op=ALU.mult), reads=[bbo, b_rc], writes=[b_om[o]])
                    P.dma("sync", "dma_start", A(out=k.omT_d[h * 128:(h + 1) * 128, tb + q0:tb + q0 + 512], in_=om[o][:]), reads=[b_om[o]])
                    banks.release(bbo); banks.release(bbs)
        P.flush()


def phase_C(k):
    nc, P, banks = k.nc, k.P, k.banks
    S = k.S; NKT = S // 128
    scale = 64.0 ** -0.5
    with ExitStack() as st:
        def sb(name, shape, dt):
            return st.enter_context(nc.sbuf_tensor(name, list(shape), dt))
        rt = sb("c_rt", [32, 16], F32); rtb = sb("c_rtb", [32, 16], BF16); ohb = sb("c_ohb", [32, 128], BF16)
        tp = sb("c_tp", [16, 512], F32); b_s = Buf()
        biasC = sb("c_biasC", [128, 16, 128], F32); biasP = sb("c_biasP", [128, 16, 128], F32); b_bias = Buf()
        sk = sb("c_sk", [64, 16], F32); b_sk = Buf()
        biasCb = sb("c_biasCb", [128, 16, 128], BF16); biasPb = sb("c_biasPb", [128, 16, 128], BF16); b_biasb = Buf()
        P.dma("sync", "dma_start", A(out=rt[:], in_=k.rel_table), writes=[b_s])
        P.op("vector", "tensor_copy", A(out=rtb[:], in_=rt[:]), reads=[b_s], writes=[b_s])
        P.op("vector", "tensor_copy", A(out=ohb[:], in_=k.cst[0:32, 257:385]), reads=[k.b_cst], writes=[b_s])
        P.op("vector", "memset", A(tp[:], NEG), writes=[b_s])
        bk, bb = banks.get()
        P.op("tensor", "matmul", A(bk[0:16, 0:128], lhsT=rtb[:], rhs=ohb[:], start=True, stop=True), reads=[b_s], writes=[bb])
        P.op("vector", "tensor_copy", A(out=tp[:, 128:256], in_=bk[0:16, 0:128]), reads=[bb, b_s], writes=[b_s])
        tw = P.dma("sync", "dma_start", A(out=k.tpad_d, in_=tp[:]), reads=[b_s])
        trep = sb("c_trep", [128, 16, 512], F32); b_tr = Buf()
        P.dma("sync", "dma_start", A(out=trep[:], in_=bass.AP(k.tpad_d.tensor, k.tpad_d.offset, [[0, 128], [512, 16], [1, 512]])), writes=[b_tr], deps=[tw])
        ZS = 128 * 513
        for h in range(16):
            zw = P.dma("sync", "dma_start", A(out=bass.AP(k.zf_d.tensor, k.zf_d.offset + h * ZS, [[513, 128], [1, 512]]), in_=trep[:, h, :]), reads=[b_tr])
            apC = bass.AP(k.zf_d.tensor, k.zf_d.offset + h * ZS + 128, [[512, 128], [1, 128]])
            apP = bass.AP(k.zf_d.tensor, k.zf_d.offset + h * ZS + 256, [[512, 128], [1, 128]])
            P.dma("sync", "dma_start", A(out=biasC[:, h, :], in_=apC), writes=[b_bias], deps=[zw])
            P.dma("sync", "dma_start", A(out=biasP[:, h, :], in_=apP), writes=[b_bias], deps=[zw])
        P.op("vector", "tensor_scalar", A(out=biasCb[:], in0=biasC[:], scalar1=1.0 / scale, scalar2=None, op0=ALU.mult), reads=[b_bias], writes=[b_biasb])
        P.op("vector", "tensor_scalar", A(out=biasPb[:], in0=biasP[:], scalar1=1.0 / scale, scalar2=None, op0=ALU.mult), reads=[b_bias], joins=[b_biasb])
        sink_bc = bass.AP(k.sinks.tensor, k.sinks.offset, [[0, 64], [1, 16]])
        P.dma("sync", "dma_start", A(out=sk[:], in_=sink_bc), writes=[b_sk])
        P.op("scalar", "activation", A(out=sk[:], in_=sk[:], func=AF.Exp), reads=[b_sk], writes=[b_sk])
        qs = [sb(f"c_qs{i}", [64, 4, S], BF16) for i in range(2)]; ks = [sb(f"c_ks{i}", [64, S], BF16) for i in range(2)]
        vs = [sb(f"c_vs{i}", [128, NKT, 64], BF16) for i in range(2)]; b_in = [Buf(), Buf()]
        rc = sb("c_rc", [64, 4, 128], F32); b_rc = Buf()
        osb = [sb(f"c_os{i}", [64, 4, 128], BF16) for i in range(2)]; b_os = [Buf(), Buf()]
        NETC = 8; NTMP = 6
        et = [sb(f"c_et{i}", [128, 512], BF16) for i in range(NETC)]; b_et = [Buf() for _ in range(NETC)]
        it = 0; cnt = {"t": 0, "e": 0, "o": 0}
        iters = []
        for sq_ in range(k.NSEQ):
            for g in range(4):
                for qi in range(NKT):
                    iters.append((sq_, g, qi))
        loaded = {}
        def ensure_loaded(sq_, g):
            nonlocal it
            if (sq_, g) in loaded: return loaded[(sq_, g)]
            i = it % 2; it += 1
            tb = sq_ * S
            P.dma("sync", "dma_start", A(out=qs[i][:], in_=k.qs_d[g * 256:(g + 1) * 256, tb:tb + S].rearrange("(h p) t -> p h t", p=64)), writes=[b_in[i]])
            P.dma("sync", "dma_start", A(out=ks[i][:], in_=k.ks_d[g * 64:(g + 1) * 64, tb:tb + S]), joins=[b_in[i]])
            P.dma("sync", "dma_start", A(out=vs[i][:], in_=k.vs_d[tb:tb + S, g * 64:(g + 1) * 64].rearrange("(j p) n -> p j n", p=128)), joins=[b_in[i]])
            loaded[(sq_, g)] = i
            return i
        staged = {}
        def stage1(n):
            sq_, g, qi = iters[n]
            i = ensure_loaded(sq_, g)
            blocks = ([(qi - 1, biasPb)] if qi > 0 else []) + [(qi, biasCb)]
            res = []
            for (kj, bias) in blocks:
                bs_, bbs_ = banks.get()
                P.op("tensor", "matmul", A(bs_[:, :].rearrange("p (h q) -> p h q", q=128), lhsT=ks[i][:, kj * 128:(kj + 1) * 128], rhs=qs[i][:, :, qi * 128:(qi + 1) * 128], start=True, stop=False),
                     reads=[b_in[i]], writes=[bbs_], signal=False)
                P.op("tensor", "matmul", A(bs_[:, :].rearrange("p (h q) -> p h q", q=128), lhsT=k.identb[:], rhs=bias[:, 4 * g:4 * g + 4, :], start=False, stop=True),
                     reads=[k.b_id, b_biasb], writes=[bbs_])
                e = cnt["e"] % NETC; cnt["e"] += 1
                P.op("scalar", "activation", A(out=et[e][:], in_=bs_[:, :], func=AF.Exp, scale=scale), reads=[bbs_], writes=[b_et[e]])
                res.append((kj, e))
            staged[n] = (i, res)
        def stage2(n):
            sq_, g, qi = iters[n]
            tb = sq_ * S
            i, res = staged.pop(n)
            bo, bbo = banks.get(hold=True); bsum, bbs = banks.get(hold=True)
            for bi, (kj, e) in enumerate(res):
                first = (bi == 0); last = (bi == len(res) - 1)
                P.op("tensor", "matmul", A(bo[0:64, :], lhsT=vs[i][:, kj, :], rhs=et[e][:], start=first, stop=last), reads=[b_in[i], b_et[e]], writes=[bbo], signal=False)
                P.op("tensor", "matmul", A(bsum[0:64, :], lhsT=k.onesb[:, 0:64], rhs=et[e][:], start=first, stop=last), reads=[k.b_id, b_et[e]], writes=[bbs], signal=True)
            P.op("vector", "tensor_tensor", A(out=rc[:], in0=bsum[0:64, :].rearrange("p (h q) -> p h q", q=128), in1=sk[:, 4 * g:4 * g + 4].unsqueeze(2).broadcast_to([64, 4, 128]), op=ALU.add),
                 reads=[bbs, b_sk], writes=[b_rc])
            P.op("vector", "reciprocal", A(out=rc[:], in_=rc[:]), reads=[b_rc], writes=[b_rc])
            o = cnt["o"] % 2; cnt["o"] += 1
            P.op("vector", "tensor_tensor", A(out=osb[o][:], in0=bo[0:64, :].rearrange("p (h q) -> p h q", q=128), in1=rc[:], op=ALU.mult), reads=[bbo, b_rc], writes=[b_os[o]])
            P.dma("sync", "dma_start", A(out=k.osT_d[g * 256:(g + 1) * 256, tb + qi * 128:tb + (qi + 1) * 128].rearrange("(h p) t -> p h t", p=64), in_=osb[o][:]), reads=[b_os[o]])
            banks.release(bbo); banks.release(bbs)
        LOOK = 2
        for n in range(min(LOOK, len(iters))): stage1(n)
        for n in range(len(iters)):
            if n + LOOK < len(iters): stage1(n + LOOK)
            stage2(n)
        P.flush()


def phase_D(k):
    nc, P, banks = k.nc, k.P, k.banks
    with ExitStack() as st:
        def sb(name, shape, dt):
            return st.enter_context(nc.sbuf_tensor(name, list(shape), dt))
        xT = sb("d_xT", [128, KC, 512], BF16); om = sb("d_om", [128, 8, 512], BF16); osb = sb("d_os", [128, 8, 512], BF16); b_act = Buf()
        bg = sb("d_bg", [128, 32], F32); b_bg = Buf()
        P.dma("sync", "dma_start", A(out=bg[:], in_=k.b_gate.rearrange("(c p) -> p c", p=128), allow_slow_non_contiguous=True), writes=[b_bg])
        wg = [sb(f"d_wg{i}", [128, 2, KC, 128], BF16) for i in range(2)]
        wb = [sb(f"d_wb{i}", [128, 2, 8, 128], BF16) for i in range(2)]; b_w = [Buf(), Buf()]
        s0 = sb("d_s0", [128, 512], F32); s1 = sb("d_s1", [128, 512], F32); b_s0 = Buf(); b_s1 = Buf()
        m1 = sb("d_m1", [128, 512], F32); m2 = sb("d_m2", [128, 512], F32); b_m1 = Buf(); b_m2 = Buf()
        mT = sb("d_mT", [128, KC, 512], BF16); b_mT = Buf()
        wi = 0
        for g in range(k.NG):
            t0 = g * 512
            P.dma("sync", "dma_start", A(out=xT[:], in_=k.xT_d[:, t0:t0 + 512].rearrange("(c p) t -> p c t", p=128)), writes=[b_act])
            P.dma("sync", "dma_start", A(out=om[:], in_=k.omT_d[:, t0:t0 + 512].rearrange("(c p) t -> p c t", p=128)), writes=[b_act])
            P.dma("sync", "dma_start", A(out=osb[:], in_=k.osT_d[:, t0:t0 + 512].rearrange("(c p) t -> p c t", p=128)), writes=[b_act])
            for c in range(KC):
                i = wi % 2; wi += 1
                P.dma("sync", "dma_start", A(out=wg[i][:, 0], in_=k.wg_d[c]), writes=[b_w[i]])
                P.dma("sync", "dma_start", A(out=wg[i][:, 1], in_=k.wg_d[KC + c]), writes=[b_w[i]])
                P.dma("sync", "dma_start", A(out=wb[i][:, 0], in_=k.wbm_d[c]), writes=[b_w[i]])
                P.dma("sync", "dma_start", A(out=wb[i][:, 1], in_=k.wbs_d[c]), writes=[b_w[i]])
                bym, bbym = banks.get(); bys, bbys = banks.get(); bg0, bbg0 = banks.get(); bg1, bbg1 = banks.get()
                def acc(out_ap, pairs, reads, b_out):
                    n = len(pairs)
                    for ii, (l, r) in enumerate(pairs):
                        P.op("tensor", "matmul", A(out_ap, lhsT=l, rhs=r, start=(ii == 0), stop=(ii == n - 1)), reads=reads, writes=[b_out], signal=(ii == n - 1))
                acc(bg0[:, :], [(wg[i][:, 0, kk, :], xT[:, kk, :]) for kk in range(KC)], [b_w[i], b_act], bbg0)
                acc(bg1[:, :], [(wg[i][:, 1, kk, :], xT[:, kk, :]) for kk in range(KC)], [b_w[i], b_act], bbg1)
                acc(bym[:, :], [(wb[i][:, 0, kk, :], om[:, kk, :]) for kk in range(8)], [b_w[i], b_act], bbym)
                acc(bys[:, :], [(wb[i][:, 1, kk, :], osb[:, kk, :]) for kk in range(8)], [b_w[i], b_act], bbys)
                P.op("scalar", "activation", A(out=s0[:], in_=bg0[:, :], func=AF.Sigmoid, bias=bg[:, c:c + 1]), reads=[bbg0, b_bg], writes=[b_s0])
                P.op("scalar", "activation", A(out=s1[:], in_=bg1[:, :], func=AF.Sigmoid, bias=bg[:, KC + c:KC + c + 1]), reads=[bbg1, b_bg], writes=[b_s1])
                P.op("vector", "tensor_tensor", A(out=m1[:], in0=bym[:, :], in1=s0[:], op=ALU.mult), reads=[bbym, b_s0], writes=[b_m1])
                P.op("vector", "tensor_tensor", A(out=m2[:], in0=bys[:, :], in1=s1[:], op=ALU.mult), reads=[bbys, b_s1], writes=[b_m2])
                P.op("gpsimd", "tensor_tensor", A(out=mT[:, c, :], in0=m1[:], in1=m2[:], op=ALU.add), reads=[b_m1, b_m2], writes=[b_mT])
            P.dma("sync", "dma_start", A(out=k.mT_d[:, t0:t0 + 512].rearrange("(c p) t -> p c t", p=128), in_=mT[:]), reads=[b_mT])
        P.flush()


def layer_norm_rows(P, k, pre, b_pre, out, b_out, g_t, b_t, b_gb, stats, mv, b_st, eng2="gpsimd"):
    for c in range(4):
        P.op("vector", "bn_stats", A(out=stats[:, c, :], in_=pre[:, c * 512:(c + 1) * 512]), reads=[b_pre], writes=[b_st])
    P.op("vector", "bn_aggr", A(out=mv[:, 0:2], in_=stats[:].rearrange("p c s -> p (c s)")), reads=[b_st], writes=[b_st])
    P.op("scalar", "activation", A(out=mv[:, 2:3], in_=mv[:, 1:2], func=AF.Sqrt, bias=k.epsl[:]), reads=[b_st, k.b_id], writes=[b_st])
    P.op("vector", "reciprocal", A(out=mv[:, 2:3], in_=mv[:, 2:3]), reads=[b_st], writes=[b_st])
    P.op("vector", "tensor_scalar", A(out=pre[:], in0=pre[:], scalar1=mv[:, 0:1], scalar2=mv[:, 2:3], op0=ALU.subtract, op1=ALU.mult), reads=[b_st], writes=[b_pre])
    P.op(eng2, "tensor_tensor", A(out=pre[:], in0=pre[:], in1=g_t[:], op=ALU.mult), reads=[b_gb], writes=[b_pre])
    P.op(eng2, "tensor_tensor", A(out=out[:], in0=pre[:], in1=b_t[:], op=ALU.add), reads=[b_gb, b_pre], writes=[b_out])


def phase_E(k):
    nc, P, banks = k.nc, k.P, k.banks
    with ExitStack() as st:
        def sb(name, shape, dt):
            return st.enter_context(nc.sbuf_tensor(name, list(shape), dt))
        wout = sb("e_wout", [128, KC, D], BF16); wr = sb("e_wr", [128, KC, NE], BF16); b_w = Buf()
        P.dma("sync", "dma_start", A(out=wout[:], in_=k.wb_out.rearrange("(c p) n -> p c n", p=128)), writes=[b_w])
        P.dma("sync", "dma_start", A(out=wr[:], in_=k.wb_router.rearrange("(c p) n -> p c n", p=128)), writes=[b_w])
        g_t = sb("e_g", [128, D], F32); b_t = sb("e_b", [128, D], F32); rb = sb("e_rb", [128, NE], F32); b_gb = Buf()
        for dst, src, n in ((g_t, k.ln1_g, D), (b_t, k.ln1_b, D), (rb, k.router_bias, NE)):
            P.dma("sync", "dma_start", A(out=dst[:], in_=bass.AP(src.tensor, src.offset, [[0, 128], [1, n]])), writes=[b_gb])
        mT = sb("e_mT", [128, KC, 512], BF16); b_mT = Buf()
        xrows = [sb(f"e_x{i}", [128, D], F32) for i in range(2)]; b_xs = [Buf(), Buf()]
        pres = [sb(f"e_pre{i}", [128, D], F32) for i in range(2)]; b_pres = [Buf(), Buf()]
        hrow = [sb(f"e_h{i}", [128, D], F32) for i in range(2)]; b_h = [Buf(), Buf()]
        hb = [sb(f"e_hb{i}", [128, D], BF16) for i in range(2)]; b_hb = [Buf(), Buf()]
        hT = sb("e_hT", [128, KC, 512], BF16); b_hT = Buf()
        stats = sb("e_stats", [128, 4, 6], F32); mv = sb("e_mv", [128, 4], F32); b_st = Buf()
        sc = sb("e_sc", [128, NE], F32); sel = sb("e_sel", [128, NE], F32); r1 = sb("e_r1", [128, NE], F32); r2 = sb("e_r2", [128, NE], F32)
        g1 = sb("e_g1", [128, 8], F32); g2 = sb("e_g2", [128, 8], F32); t8 = sb("e_t8", [128, 8], F32); gm = sb("e_gm", [128, 8], F32)
        ws = sb("e_ws", [128, 2], F32); b_r = Buf()
        hi = 0
        for g in range(k.NG):
            P.dma("sync", "dma_start", A(out=mT[:], in_=k.mT_d[:, g * 512:(g + 1) * 512].rearrange("(c p) t -> p c t", p=128)), writes=[b_mT])
            for tt in range(4):
                t = g * 4 + tt; r0 = t * 128
                xrow = xrows[t % 2]; b_x = b_xs[t % 2]; pre = pres[t % 2]; b_pre = b_pres[t % 2]
                P.dma("sync", "dma_start", A(out=xrow[:], in_=k.x[r0:r0 + 128, :]), writes=[b_x])
                for n in range(4):
                    bk, bb = banks.get()
                    for kk in range(KC):
                        P.op("tensor", "matmul", A(bk[:, :], lhsT=mT[:, kk, tt * 128:(tt + 1) * 128], rhs=wout[:, kk, n * 512:(n + 1) * 512], start=(kk == 0), stop=(kk == KC - 1)),
                             reads=[b_mT, b_w], writes=[bb], signal=(kk == KC - 1))
                    P.op("vector", "scalar_tensor_tensor", A(out=pre[:, n * 512:(n + 1) * 512], in0=xrow[:, n * 512:(n + 1) * 512], scalar=ALPHA, in1=bk[:, :], op0=ALU.mult, op1=ALU.add),
                         reads=[b_x, bb], writes=[b_pre])
                i = hi % 2; hi += 1
                layer_norm_rows(P, k, pre, b_pre, hrow[i], b_h[i], g_t, b_t, b_gb, stats, mv, b_st)
                P.dma("sync", "dma_start", A(out=k.h_d[r0:r0 + 128, :], in_=hrow[i][:]), reads=[b_h[i]])
                P.op("scalar", "activation", A(out=hb[i][:], in_=hrow[i][:], func=AF.Copy), reads=[b_h[i]], writes=[b_hb[i]])
                P.dma("sync", "dma_start", A(out=k.hb_d[r0:r0 + 128, :], in_=hb[i][:]), reads=[b_hb[i]])
                for half in range(2):
                    bk, bb = banks.get()
                    bkb = bk[:, :].bitcast(BF16)
                    for j in range(8):
                        c = half * 8 + j
                        P.op("tensor", "transpose", A(out=bkb[:, j * 128:(j + 1) * 128], in_=hb[i][:, c * 128:(c + 1) * 128], identity=k.identb[:]),
                             reads=[b_hb[i], k.b_id], writes=[bb], signal=(j == 7))
                    P.op("vector", "tensor_copy", A(out=hT[:, half * 8:(half + 1) * 8, tt * 128:(tt + 1) * 128], in_=bkb.rearrange("p (c t) -> p c t", t=128)), reads=[bb], writes=[b_hT])
                bk, bb = banks.get()
                for kk in range(KC):
                    P.op("tensor", "matmul", A(bk[:, 0:NE], lhsT=hT[:, kk, tt * 128:(tt + 1) * 128], rhs=wr[:, kk, :], start=(kk == 0), stop=(kk == KC - 1)),
                         reads=[b_hT, b_w], writes=[bb], signal=(kk == KC - 1))
                P.op("scalar", "activation", A(out=sc[:], in_=bk[:, 0:NE], func=AF.Sigmoid), reads=[bb], writes=[b_r])
                V = lambda name, a, **kw: P.op("vector", name, a, reads=[b_r] + kw.get("reads", []), writes=[b_r] + kw.get("writes", []))
                sel3 = sel[:].rearrange("p (g e) -> p g e", e=8); r13 = r1[:].rearrange("p (g e) -> p g e", e=8); r23 = r2[:].rearrange("p (g e) -> p g e", e=8)
                V("tensor_tensor", A(out=sel[:], in0=sc[:], in1=rb[:], op=ALU.add), reads=[b_gb])
                V("tensor_reduce", A(out=g1[:], in_=sel3, axis=AX.X, op=ALU.max))
                V("tensor_tensor", A(out=r13, in0=sel3, in1=g1[:].unsqueeze(2).broadcast_to([128, 8, 8]), op=ALU.is_equal))
                V("scalar_tensor_tensor", A(out=r2[:], in0=r1[:], scalar=-1.0e9, in1=sel[:], op0=ALU.mult, op1=ALU.add))
                V("tensor_reduce", A(out=g2[:], in_=r23, axis=AX.X, op=ALU.max))
                V("tensor_tensor", A(out=g1[:], in0=g1[:], in1=g2[:], op=ALU.add))
                V("max", A(out=t8[:], in_=g1[:]))
                V("tensor_scalar", A(out=gm[:], in0=g1[:], scalar1=t8[:, 3:4], scalar2=None, op0=ALU.is_ge))
                V("scalar_tensor_tensor", A(out=r13, in0=sel3, scalar=2.0, in1=gm[:].unsqueeze(2).broadcast_to([128, 8, 8]), op0=ALU.add, op1=ALU.mult))
                V("max", A(out=t8[:], in_=r1[:]))
                V("tensor_scalar", A(out=k.Msk[:, t, :], in0=r1[:], scalar1=t8[:, 7:8], scalar2=None, op0=ALU.is_ge), writes=[k.b_Msk])
                V("tensor_tensor", A(out=r2[:], in0=sc[:], in1=k.Msk[:, t, :], op=ALU.mult))
                V("tensor_reduce", A(out=ws[:, 0:1], in_=r2[:], axis=AX.X, op=ALU.add))
                V("reciprocal", A(out=ws[:, 1:2], in_=ws[:, 0:1]))
                V("tensor_scalar", A(out=k.Wc[:, t, :], in0=r2[:], scalar1=ws[:, 1:2], scalar2=2.5, op0=ALU.mult, op1=ALU.mult), writes=[k.b_Wc])
            P.dma("sync", "dma_start", A(out=k.hT_d[:, g * 512:(g + 1) * 512].rearrange("(c p) t -> p c t", p=128), in_=hT[:]), reads=[b_hT])
        if "dbg_route" in k.debug:
            P.dma("sync", "dma_start", A(out=k.dbg_route[:, 0:k.NT * NE], in_=k.Wc[:].rearrange("p t e -> p (t e)")), reads=[k.b_Wc])
        P.flush()


def phase_FG(k):
    nc, P, banks = k.nc, k.P, k.banks
    NT, CAP = k.NT, k.CAP
    NC_ = NT * NE
    with ExitStack() as st:
        def sb(name, shape, dt):
            return st.enter_context(nc.sbuf_tensor(name, list(shape), dt))
        Mb = sb("f_Mb", [128, NC_], BF16); us = sb("f_us", [128, 128], BF16)
        pos = sb("f_pos", [128, NT, NE], F32); tot = sb("f_tot", [128, NT, NE], F32); base = sb("f_base", [128, NT, NE], F32)
        key = sb("f_key", [128, NT, NE], F32); k8 = sb("f_k8", [128, NT, 8], F32); iz = sb("f_iz", [128, NT, 8], F32)
        ecapi = sb("f_ecapi", [128, NE], I32); ecap = sb("f_ecap", [128, NE], F32); dump1 = sb("f_dump1", [128, 1], F32)
        junk = sb("f_junk", [128, NE], F32)
        b = Buf()
        V = lambda name, a, **kw: P.op("vector", name, a, reads=[b] + kw.get("reads", []), writes=[b] + kw.get("writes", []))
        V("tensor_copy", A(out=Mb[:], in_=k.Msk[:].rearrange("p t e -> p (t e)")), reads=[k.b_Msk])
        V("tensor_tensor", A(out=us[:], in0=k.trib[:], in1=k.identb[:], op=ALU.subtract), reads=[k.b_id])
        posf = pos[:].rearrange("p t e -> p (t e)"); totf = tot[:].rearrange("p t e -> p (t e)")
        for c0 in range(0, NC_, 512):
            cw = min(512, NC_ - c0)
            bk, bb = banks.get()
            P.op("tensor", "matmul", A(bk[:, 0:cw], lhsT=us[:], rhs=Mb[:, c0:c0 + cw], start=True, stop=True), reads=[b], writes=[bb])
            P.op("vector", "tensor_copy", A(out=posf[:, c0:c0 + cw], in_=bk[:, 0:cw]), reads=[bb, b], writes=[b])
            bk, bb = banks.get()
            P.op("tensor", "matmul", A(bk[:, 0:cw], lhsT=k.onesb[:], rhs=Mb[:, c0:c0 + cw], start=True, stop=True), reads=[b, k.b_id], writes=[bb])
            P.op("vector", "tensor_copy", A(out=totf[:, c0:c0 + cw], in_=bk[:, 0:cw]), reads=[bb, b], writes=[b])
        V("memset", A(base[:, 0, :], 0.0))
        for t in range(1, NT):
            V("tensor_tensor", A(out=base[:, t, :], in0=base[:, t - 1, :], in1=tot[:, t - 1, :], op=ALU.add))
        V("tensor_tensor", A(out=pos[:], in0=pos[:], in1=base[:], op=ALU.add))
        V("scalar_tensor_tensor", A(out=tot[:], in0=pos[:], scalar=float(CAP), in1=k.Msk[:], op0=ALU.is_lt, op1=ALU.mult), reads=[k.b_Msk])
        P.op("gpsimd", "iota", A(ecapi[:], pattern=[[CAP, NE]], base=1, channel_multiplier=0), reads=[b], writes=[b])
        V("tensor_copy", A(out=ecap[:], in_=ecapi[:]))
        V("tensor_tensor", A(out=key[:], in0=pos[:], in1=ecap[:].unsqueeze(1).broadcast_to([128, NT, NE]), op=ALU.add))
        V("tensor_tensor", A(out=key[:], in0=key[:], in1=tot[:], op=ALU.mult))
        for t in range(NT):
            V("max", A(out=k8[:, t, :], in_=key[:, t, :]))
        V("tensor_scalar", A(out=dump1[:], in0=k.cst[:, 385:386], scalar1=float(NE * CAP + 1), scalar2=None, op0=ALU.add), reads=[k.b_cst])
        V("tensor_scalar", A(out=iz[:], in0=k8[:], scalar1=0.0, scalar2=None, op0=ALU.is_equal))
        V("scalar_tensor_tensor", A(out=iz[:], in0=iz[:], scalar=dump1[:, 0:1], in1=k8[:], op0=ALU.mult, op1=ALU.add))
        V("tensor_scalar", A(out=iz[:], in0=iz[:], scalar1=-1.0, scalar2=None, op0=ALU.add))
        V("tensor_copy", A(out=k.idx[:], in_=iz[:]), writes=[k.b_idx])
        V("memset", A(k.wj[:], 0.0), writes=[k.b_wj])
        for t in range(NT):
            for j in range(8):
                V("scalar_tensor_tensor", A(out=junk[:], in0=key[:, t, :], scalar=k8[:, t, j:j + 1], in1=k.Wc[:, t, :], op0=ALU.is_equal, op1=ALU.mult, accum_out=k.wj[:, t, j:j + 1]),
                  reads=[k.b_Wc], writes=[k.b_wj])
        if "dbg_route" in k.debug:
            P.dma("sync", "dma_start", A(out=k.dbg_route[:, NT * NE:NT * NE + NT * 8], in_=iz[:].rearrange("p t j -> p (t j)")), reads=[b])
            P.dma("sync", "dma_start", A(out=k.dbg_route[:, NT * NE + NT * 8:NT * NE + NT * 16], in_=k.wj[:].rearrange("p t j -> p (t j)")), reads=[k.b_wj])
        hbr = [sb(f"f_hb{i}", [128, D], BF16) for i in range(2)]; b_hb = [Buf(), Buf()]
        first_sc = [True]
        def scatter_tile(t):
            i = t % 2
            P.dma("sync", "dma_start", A(out=hbr[i][:], in_=k.hb_d[t * 128:(t + 1) * 128, :]), writes=[b_hb[i]])
            for j in range(8):
                kw = dict(writes=[k.b_xg]) if first_sc[0] else dict(joins=[k.b_xg])
                first_sc[0] = False
                P.dma("gpsimd", "indirect_dma_start", A(out=k.xg_d, out_offset=bass.IndirectOffsetOnAxis(ap=k.idx[:, t, j:j + 1], axis=0), in_=hbr[i][:], in_offset=None),
                      reads=[b_hb[i], k.b_idx], **kw)
        wgu = sb("g_wgu", [128, KC, 1024], BF16); wd = sb("g_wd", [128, 4, D], BF16); b_w = Buf()
        P.dma("sync", "dma_start", A(out=wgu[:], in_=k.wb_sgu.rearrange("(c p) n -> p c n", p=128)), writes=[b_w])
        P.dma("sync", "dma_start", A(out=wd[:], in_=k.wb_sd.rearrange("(c p) n -> p c n", p=128)), writes=[b_w])
        hT = [sb(f"g_hT{i}", [128, KC, 512], BF16) for i in range(2)]; b_hT = [Buf(), Buf()]
        aT = sb("g_aT", [128, 4, 512], BF16); b_aT = Buf()
        tmp = [sb(f"g_tmp{i}", [128, 512], F32) for i in range(2)]; b_tmp = [Buf(), Buf()]
        yrow = [sb(f"g_y{i}", [128, D], F32) for i in range(2)]; b_y = [Buf(), Buf()]
        yi = 0
        for g in range(k.NG):
            i = g % 2
            P.dma("sync", "dma_start", A(out=hT[i][:], in_=k.hT_d[:, g * 512:(g + 1) * 512].rearrange("(c p) t -> p c t", p=128)), writes=[b_hT[i]])
            ffn_T(P, banks, wgu, b_w, lambda kk, n0, nw, i=i: hT[i][:, kk, n0:n0 + nw], b_hT[i], [(0, 512)], aT, b_aT, tmp, b_tmp, KC)
            for tt in range(4):
                y = yi % 2; yi += 1
                for n in range(4):
                    bk, bb = banks.get()
                    for c in range(4):
                        P.op("tensor", "matmul", A(bk[:, :], lhsT=aT[:, c, tt * 128:(tt + 1) * 128], rhs=wd[:, c, n * 512:(n + 1) * 512], start=(c == 0), stop=(c == 3)),
                             reads=[b_aT, b_w], writes=[bb], signal=(c == 3))
                    eng, nm, a = (("scalar", "activation", A(out=yrow[y][:, n * 512:(n + 1) * 512], in_=bk[:, :], func=AF.Copy)) if n % 2 == 0 else
                                  ("vector", "tensor_copy", A(out=yrow[y][:, n * 512:(n + 1) * 512], in_=bk[:, :])))
                    P.op(eng, nm, a, reads=[bb], writes=[b_y[y]])
                r0 = (g * 4 + tt) * 128
                P.dma("sync", "dma_start", A(out=k.ysh_d[r0:r0 + 128, :], in_=yrow[y][:]), reads=[b_y[y]])
                scatter_tile(g * 4 + tt)
        P.flush()


def ffn_T(P, banks, wgu, b_wgu, xT_ap, b_x, pieces, aT, b_aT, tmp, b_tmp, KCH):
    ti = 0
    for c in range(4):
        gb = [banks.get(hold=True) for _ in pieces]
        ub = [banks.get(hold=True) for _ in pieces]
        for (bl, col0) in ((gb, c * 128), (ub, 512 + c * 128)):
            for kk in range(KCH):
                for pi, (n0, nw) in enumerate(pieces):
                    bk_, bb_ = bl[pi]
                    P.op("tensor", "matmul", A(bk_[:, 0:nw], lhsT=wgu[:, kk, col0:col0 + 128], rhs=xT_ap(kk, n0, nw), start=(kk == 0), stop=(kk == KCH - 1)),
                         reads=[b_wgu, b_x], writes=[bb_], signal=(kk == KCH - 1))
        for pi, (n0, nw) in enumerate(pieces):
            i = ti % len(tmp); ti += 1
            P.op("scalar", "activation", A(out=tmp[i][:, 0:nw], in_=gb[pi][0][:, 0:nw], func=AF.Silu), reads=[gb[pi][1]], writes=[b_tmp[i]])
            P.op("vector", "tensor_tensor", A(out=aT[:, c, n0:n0 + nw], in0=ub[pi][0][:, 0:nw], in1=tmp[i][:, 0:nw], op=ALU.mult), reads=[ub[pi][1], b_tmp[i]], writes=[b_aT])
        for (_, bb_) in gb + ub: banks.release(bb_)


def phase_H(k):
    nc, P, banks = k.nc, k.P, k.banks
    CAP = k.CAP; NS = CAP // 128
    npieces = (CAP + 511) // 512; NP = CAP // npieces
    with ExitStack() as st:
        def sb(name, shape, dt):
            return st.enter_context(nc.sbuf_tensor(name, list(shape), dt))
        wgu = [sb(f"h_wgu{i}", [128, KC, 1024], BF16) for i in range(2)]; b_wgu = [Buf(), Buf()]
        wd = [sb(f"h_wd{i}", [128, 4, D], BF16) for i in range(2)]; b_wd = [Buf(), Buf()]
        NSTG = 4
        stg = [sb(f"h_stg{i}", [128, 2048], F32) for i in range(NSTG)]; b_stg = [Buf() for _ in range(NSTG)]
        NXE = NS
        xe = [sb(f"h_xe{i}", [128, D], BF16) for i in range(NXE)]; b_xe = [Buf() for _ in range(NXE)]
        xeT = sb("h_xeT", [128, KC, CAP], BF16); b_xeT = Buf()
        aT = sb("h_aT", [128, 4, CAP], BF16); b_aT = Buf()
        tmp = [sb(f"h_tmp{i}", [128, NP], F32) for i in range(3)]; b_tmp = [Buf() for _ in range(3)]
        yrow = [sb(f"h_y{i}", [128, D], BF16) for i in range(2)]; b_y = [Buf(), Buf()]
        cast_engs = ["vector", "scalar", "vector", "scalar"]
        si = [0]
        def prefetch(e):
            w = e % 2
            for pc in range(12):
                i = si[0] % NSTG; ce = cast_engs[si[0] % 4]; si[0] += 1
                if pc < 8:
                    src = k.w_gu[e, pc * 256:(pc + 1) * 256, :].rearrange("(c p) n -> p c n", p=128)
                    dst = wgu[w][:, 2 * pc:2 * pc + 2, :]; sv = stg[i][:].rearrange("p (c n) -> p c n", n=1024); bw = b_wgu[w]
                else:
                    c = pc - 8
                    src = k.w_dn[e, c * 128:(c + 1) * 128, :]
                    dst = wd[w][:, c, :]; sv = stg[i][:]; bw = b_wd[w]
                P.dma("sync", "dma_start", A(out=sv, in_=src), writes=[b_stg[i]])
                if ce == "scalar":
                    P.op("scalar", "activation", A(out=dst, in_=sv, func=AF.Copy), reads=[b_stg[i]], writes=[bw])
                else:
                    P.op(ce, "tensor_copy", A(out=dst, in_=sv), reads=[b_stg[i]], writes=[bw])
                yield
        def run_some(gen, n):
            for _ in range(n):
                try: next(gen)
                except StopIteration: return
        g0 = prefetch(0); run_some(g0, 12)
        yi = 0
        def load_xe(e, s_):
            r0 = e * CAP + s_ * 128
            P.dma("gpsimd", "dma_start", A(out=xe[s_][:], in_=k.xg_d[r0:r0 + 128, :]), reads=[k.b_xg], writes=[b_xe[s_]])
        for s_ in range(NS): load_xe(0, s_)
        for e in range(NE):
            w = e % 2
            gen = prefetch(e + 1) if e + 1 < NE else iter(())
            for s_ in range(NS):
                i = s_
                for half in range(2):
                    bk, bb = banks.get()
                    bkb = bk[:, :].bitcast(BF16)
                    for j in range(8):
                        c = half * 8 + j
                        P.op("tensor", "transpose", A(out=bkb[:, j * 128:(j + 1) * 128], in_=xe[i][:, c * 128:(c + 1) * 128], identity=k.identb[:]),
                             reads=[b_xe[i], k.b_id], writes=[bb], signal=(j == 7))
                    P.op("vector", "tensor_copy", A(out=xeT[:, half * 8:(half + 1) * 8, s_ * 128:(s_ + 1) * 128], in_=bkb.rearrange("p (c t) -> p c t", t=128)), reads=[bb], writes=[b_xeT])
                run_some(gen, 1)
            if e + 1 < NE:
                for s_ in range(NS): load_xe(e + 1, s_)
            ffn_T(P, banks, wgu[w], b_wgu[w], lambda kk, n0, nw: xeT[:, kk, n0:n0 + nw], b_xeT, [(pc * NP, NP) for pc in range(npieces)], aT, b_aT, tmp, b_tmp, KC)
            run_some(gen, 4)
            for s_ in range(NS):
                y = yi % 2; yi += 1
                for n in range(4):
                    bk, bb = banks.get()
                    for c in range(4):
                        P.op("tensor", "matmul", A(bk[:, :], lhsT=aT[:, c, s_ * 128:(s_ + 1) * 128], rhs=wd[w][:, c, n * 512:(n + 1) * 512], start=(c == 0), stop=(c == 3)),
                             reads=[b_aT, b_wd[w]], writes=[bb], signal=(c == 3))
                    eng, nm, a = (("scalar", "activation", A(out=yrow[y][:, n * 512:(n + 1) * 512], in_=bk[:, :], func=AF.Copy)) if n % 2 == 0 else
                                  ("vector", "tensor_copy", A(out=yrow[y][:, n * 512:(n + 1) * 512], in_=bk[:, :])))
                    P.op(eng, nm, a, reads=[bb], writes=[b_y[y]])
                r0 = e * CAP + s_ * 128
                P.dma("gpsimd", "dma_start", A(out=k.yg_d[r0:r0 + 128, :], in_=yrow[y][:]), reads=[b_y[y]], joins=[k.b_yg])
                run_some(gen, 2)
            run_some(gen, 12)
        P.flush()


def phase_I(k):
    nc, P, banks = k.nc, k.P, k.banks
    with ExitStack() as st:
        def sb(name, shape, dt):
            return st.enter_context(nc.sbuf_tensor(name, list(shape), dt))
        g_t = sb("i_g", [128, D], F32); b_t = sb("i_b", [128, D], F32); b_gb = Buf()
        for dst, src in ((g_t, k.ln2_g), (b_t, k.ln2_b)):
            P.dma("sync", "dma_start", A(out=dst[:], in_=bass.AP(src.tensor, src.offset, [[0, 128], [1, D]])), joins=[b_gb])
        hrow = [sb(f"i_h{i}", [128, D], F32) for i in range(2)]; ysh = [sb(f"i_ys{i}", [128, D], F32) for i in range(2)]; b_in = [Buf(), Buf()]
        NY = 12
        yj = [sb(f"i_yj{i}", [128, D], BF16) for i in range(NY)]; b_yj = [Buf() for _ in range(NY)]
        dg = [sb(f"i_dg{i}", [128, 128], BF16) for i in range(NY)]; b_dg = [Buf() for _ in range(NY)]
        acc = [sb(f"i_acc{i}", [128, D], F32) for i in range(2)]; b_acc = [Buf(), Buf()]
        orow = [sb(f"i_o{i}", [128, D], F32) for i in range(2)]; b_o = [Buf(), Buf()]
        stats = sb("i_stats", [128, 4, 6], F32); mv = sb("i_mv", [128, 4], F32); b_st = Buf()
        ji = 0
        for t in range(k.NT):
            i = t % 2; r0 = t * 128
            P.dma("sync", "dma_start", A(out=hrow[i][:], in_=k.h_d[r0:r0 + 128, :]), writes=[b_in[i]])
            P.dma("sync", "dma_start", A(out=ysh[i][:], in_=k.ysh_d[r0:r0 + 128, :]), joins=[b_in[i]])
            P.op("vector", "scalar_tensor_tensor", A(out=acc[i][:], in0=hrow[i][:], scalar=ALPHA, in1=ysh[i][:], op0=ALU.mult, op1=ALU.add), reads=[b_in[i]], writes=[b_acc[i]])
            bks = [banks.get(hold=True) for _ in range(4)]
            for j in range(8):
                q = ji % NY; ji += 1
                P.dma("gpsimd", "indirect_dma_start", A(out=yj[q][:], out_offset=None, in_=k.yg_d, in_offset=bass.IndirectOffsetOnAxis(ap=k.idx[:, t, j:j + 1], axis=0)),
                      reads=[k.b_yg, k.b_idx], writes=[b_yj[q]])
                P.op("vector", "tensor_scalar", A(out=dg[q][:], in0=k.identb[:], scalar1=k.wj[:, t, j:j + 1], scalar2=None, op0=ALU.mult), reads=[k.b_id, k.b_wj], writes=[b_dg[q]])
                for n in range(4):
                    P.op("tensor", "matmul", A(bks[n][0][:, :], lhsT=dg[q][:], rhs=yj[q][:, n * 512:(n + 1) * 512], start=(j == 0), stop=(j == 7)),
                         reads=[b_dg[q], b_yj[q]], writes=[bks[n][1]], signal=(j == 7 or n == 3))
            for n in range(4):
                P.op("vector", "tensor_tensor", A(out=acc[i][:, n * 512:(n + 1) * 512], in0=acc[i][:, n * 512:(n + 1) * 512], in1=bks[n][0][:, :], op=ALU.add),
                     reads=[bks[n][1]], writes=[b_acc[i]])
                banks.release(bks[n][1])
            layer_norm_rows(P, k, acc[i], b_acc[i], orow[i], b_o[i], g_t, b_t, b_gb, stats, mv, b_st, eng2="vector")
            P.dma("sync", "dma_start", A(out=k.out[r0:r0 + 128, :], in_=orow[i][:]), reads=[b_o[i]])
        P.flush()


class K:
    pass


def build_program(NSEQ, S, CAP, debug=(), upto=99):
    T = NSEQ * S
    NG = T // 512
    NT = T // 128
    nc = bass.Bass("TRN2", target_bir_lowering=False)
    k = K(); k.upto = upto; k.debug = debug; k.nc = nc; k.T = T; k.S = S; k.NSEQ = NSEQ; k.NG = NG; k.NT = NT; k.CAP = CAP
    def din(name, shape, dt=F32):
        return nc.dram_tensor(name, list(shape), dt, kind="ExternalInput").ap()
    def dscr(name, shape, dt):
        kind = "ExternalOutput" if name in debug else "Internal"
        return nc.dram_tensor(name, list(shape), dt, kind=kind).ap()
    k.x = din("x", [T, D]); k.pos = din("positions", [T], I32)
    k.w_in = din("w_in", [D, IN_COLS]); k.b_gate = din("b_gate", [2 * D])
    k.q_norm_g = din("q_norm_g", [512]); k.kv_norm_g = din("kv_norm_g", [256])
    k.w_uq = din("w_uq", [512, 1536]); k.w_uk = din("w_uk", [256, 1024]); k.w_uv = din("w_uv", [256, 1024])
    k.sinks = din("swa_sinks", [16]); k.rel_table = din("rel_table", [32, 16])
    k.w_br_mla = din("w_br_mla", [1024, D]); k.w_br_swa = din("w_br_swa", [1024, D]); k.w_out = din("w_out", [D, D])
    k.ln1_g = din("ln1_g", [D]); k.ln1_b = din("ln1_b", [D])
    k.w_router = din("w_router", [D, NE]); k.router_bias = din("router_bias", [NE])
    k.w_gu = din("w_gate_up", [NE, D, 2 * FF]); k.w_dn = din("w_down", [NE, FF, D])
    k.w_sgu = din("w_shared_gate_up", [D, 2 * FF]); k.w_sd = din("w_shared_down", [FF, D])
    k.ln2_g = din("ln2_g", [D]); k.ln2_b = din("ln2_b", [D])
    k.consts = din("consts", [128, 512])
    k.out = nc.dram_tensor("out", [T, D], F32, kind="ExternalOutput").ap()
    k.wb_in = dscr("wb_in", [D, IN_COLS], BF16)
    k.wb_krrot = dscr("wb_krrot", [D, 64], BF16)
    k.wb_uq = dscr("wb_uq", [512, 1536], BF16); k.wb_uqrot = dscr("wb_uqrot", [512, 512], BF16)
    k.wb_uk = dscr("wb_uk", [256, 1024], BF16); k.wb_uv = dscr("wb_uv", [256, 1024], BF16)
    k.wb_brm = dscr("wb_brm", [1024, D], BF16); k.wb_brs = dscr("wb_brs", [1024, D], BF16)
    k.wb_out = dscr("wb_out", [D, D], BF16); k.wb_router = dscr("wb_router", [D, NE], BF16)
    k.wg_d = dscr("wg_d", [32, 128, KC, 128], BF16); k.wbm_d = dscr("wbm_d", [KC, 128, 8, 128], BF16); k.wbs_d = dscr("wbs_d", [KC, 128, 8, 128], BF16)
    k.wb_sgu = dscr("wb_sgu", [D, 2 * FF], BF16); k.wb_sd = dscr("wb_sd", [FF, D], BF16)
    k.xT_d = dscr("xT_d", [D, T], BF16)
    k.qn_d = dscr("qn_d", [1024, T], BF16); k.qr_d = dscr("qr_d", [512, T], BF16)
    k.kn_d = dscr("kn_d", [1024, T], BF16); k.kr_d = dscr("kr_d", [64, T], BF16)
    k.vm_d = dscr("vm_d", [T, 1024], BF16)
    k.qs_d = dscr("qs_d", [1024, T], BF16); k.ks_d = dscr("ks_d", [256, T], BF16); k.vs_d = dscr("vs_d", [T, 256], BF16)
    k.omT_d = dscr("omT_d", [1024, T], BF16); k.osT_d = dscr("osT_d", [1024, T], BF16)
    k.mT_d = dscr("mT_d", [D, T], BF16)
    k.h_d = dscr("h_d", [T, D], F32); k.hb_d = dscr("hb_d", [T, D], BF16); k.hT_d = dscr("hT_d", [D, T], BF16)
    k.tpad_d = dscr("tpad_d", [16, 512], F32)
    k.zf_d = dscr("zf_d", [16 * 128 * 513], F32)
    k.ysh_d = dscr("ysh_d", [T, D], F32)
    k.xg_d = dscr("xg_d", [NE * CAP + 128, D], BF16)
    k.yg_d = dscr("yg_d", [NE * CAP + 128, D], BF16)
    k.dbg_route = dscr("dbg_route", [128, NT * 80], F32)

    with ExitStack() as st:
        P = Prog(nc, st); k.P = P
        k.banks = PsumBanks(nc, st)
        def sb(name, shape, dt):
            return st.enter_context(nc.sbuf_tensor(name, list(shape), dt))
        k.cst = sb("cst", [128, 512], F32); k.b_cst = Buf()
        k.identb = sb("identb", [128, 128], BF16); k.trib = sb("trib", [128, 128], BF16)
        k.onesb = sb("onesb", [128, 128], BF16); k.b_id = Buf()
        k.idx = sb("idx", [128, NT, 8], I32); k.b_idx = Buf()
        k.wj = sb("wj", [128, NT, 8], F32); k.b_wj = Buf()
        k.b_xg = Buf(); k.b_yg = Buf()
        k.epsr = sb("epsr", [128, 1], F32); k.epsl = sb("epsl", [128, 1], F32)
        P.op("vector", "memset", A(k.epsr[:], RMS_EPS), writes=[k.b_id])
        P.op("vector", "memset", A(k.epsl[:], LN_EPS), writes=[k.b_id])
        P.dma("sync", "dma_start", A(out=k.cst[:], in_=k.consts), writes=[k.b_cst])
        P.op("vector", "tensor_copy", A(out=k.identb[:], in_=k.cst[:, 0:128]), reads=[k.b_cst], writes=[k.b_id])
        P.op("vector", "tensor_copy", A(out=k.trib[:], in_=k.cst[:, 128:256]), reads=[k.b_cst], writes=[k.b_id])
        P.op("vector", "memset", A(k.onesb[:], 1.0), writes=[k.b_id])
        k.zrowb = sb("zrowb", [128, D], BF16); k.b_z = Buf()
        P.op("gpsimd", "memset", A(k.zrowb[:], 0.0), writes=[k.b_z])
        phase_W(k, 1)
        phase_A(k)
        if k.upto >= 2: phase_B(k)
        if k.upto >= 3: phase_C(k)
        if k.upto >= 4: phase_D(k)
        with ExitStack() as st_r:
            k.Wc = st_r.enter_context(nc.sbuf_tensor("Wc", [128, NT, NE], F32)); k.b_Wc = Buf()
            k.Msk = st_r.enter_context(nc.sbuf_tensor("Msk", [128, NT, NE], F32)); k.b_Msk = Buf()
            if k.upto >= 5: phase_E(k)
            if k.upto >= 6: phase_FG(k)
        if k.upto >= 8: phase_H(k)
        if k.upto >= 9: phase_I(k)
        P.flush(final=True)
    return nc


def make_in_map(inputs, b0, nseq, S, consts):
    m = {}
    m["x"] = np.ascontiguousarray(inputs["x"][b0:b0 + nseq, :S].reshape(nseq * S, D))
    m["positions"] = np.ascontiguousarray(inputs["positions"][b0:b0 + nseq, :S].reshape(nseq * S)).astype(np.int32)
    for name in ("w_in", "q_norm_g", "kv_norm_g", "w_uq", "w_uk", "w_uv", "swa_sinks", "w_br_mla", "w_br_swa", "w_out",
                 "ln1_g", "ln1_b", "w_router", "router_bias", "w_gate_up", "w_down", "w_shared_gate_up", "w_shared_down",
                 "ln2_g", "ln2_b"):
        m[name] = np.ascontiguousarray(inputs[name][0])
    m["b_gate"] = np.ascontiguousarray(inputs["b_gate"][0].reshape(-1))
    m["rel_table"] = np.ascontiguousarray(inputs["rel_table"])
    m["consts"] = consts
    return m


def kernel(**inputs):
    NCORES = 8; NSEQ = 2; S = 2048; CAP = 768
    inputs = {k_: np.asarray(v) for k_, v in inputs.items()}
    nc = build_program(NSEQ, S, CAP)
    consts = make_consts()
    in_maps = [make_in_map(inputs, c * NSEQ, NSEQ, S, consts) for c in range(NCORES)]
    res = run_bass_kernel_spmd(nc, in_maps, core_ids=list(range(NCORES)))
    outs = [np.asarray(r["out"]).reshape(NSEQ, S, D) for r in res.results]
    return np.concatenate(outs, axis=0).astype(np.float32)
```
